# Optimizing a Trainium2 kernel written in Bass

```python
import math
import jax, jax.numpy as jnp
from jax import lax
import numpy as np

D_MODEL = 2048
BATCH = 8
SEQ = 2048
DEPTH = 1

CONV_DIM = 1024
CONV_WIDTH = 3
DIL_PATTERNS = ((128, 1), (512, 4), (2048, 16))
N_DIL_GROUPS = 3
HEADS_PER_GROUP = 4
HEAD_DIM = 128
DIL_DIM = N_DIL_GROUPS * HEADS_PER_GROUP * HEAD_DIM
DIL_OUT_DIM = HEADS_PER_GROUP * HEAD_DIM
ATT_BLOCK = 128
ALIBI_MAX = 8.0
MEM_LEN = 256
MEM_HEADS = 4
MEM_HEAD_DIM = 256
MEM_DIM = MEM_HEADS * MEM_HEAD_DIM
N_BRANCHES = 3
IN_DIM = 3 * CONV_DIM + 3 * DIL_DIM + MEM_DIM + N_BRANCHES * D_MODEL
N_GROUPS = 4
EXPERTS_PER_GROUP = 8
TOP_K = 2
D_EXPERT = 512
ALPHA = (2 * DEPTH) ** 0.25
BETA = (8 * DEPTH) ** -0.25
LN_EPS = 1e-5

kernel_name = "hybrid_conv_dilattn_mem_hmoe_deepnorm"


def _in_splits():
    sizes = [CONV_DIM, CONV_DIM, CONV_DIM, DIL_DIM, DIL_DIM, DIL_DIM, MEM_DIM, N_BRANCHES * D_MODEL]
    out, acc = [], 0
    for s in sizes[:-1]:
        acc += s
        out.append(acc)
    return out


def _alibi_slopes():
    n = N_DIL_GROUPS * HEADS_PER_GROUP
    s = 2.0 ** (-ALIBI_MAX * np.arange(1, n + 1, dtype=np.float32) / n)
    return jnp.asarray(s.reshape(N_DIL_GROUPS, HEADS_PER_GROUP), dtype=jnp.float32)


def layer_norm(x, g, b):
    xf = x.astype(jnp.float32)
    mu = jnp.mean(xf, -1, keepdims=True)
    var = jnp.mean(jnp.square(xf - mu), -1, keepdims=True)
    y = (xf - mu) * lax.rsqrt(var + LN_EPS) * g.astype(jnp.float32) + b.astype(jnp.float32)
    return y.astype(x.dtype)


def short_conv(bg, cg, h, w_conv):
    u = cg * h
    S = u.shape[1]
    up = jnp.pad(u, ((0, 0), (CONV_WIDTH - 1, 0), (0, 0)))
    y = w_conv[0] * up[:, 0:S]
    for j in range(1, CONV_WIDTH):
        y = y + w_conv[j] * up[:, j:j + S]
    return bg * y


def dilated_group_attention(q, k, v, slopes, window, dilation):
    B, S, H, Dh = q.shape
    n_back = window // dilation
    L = S // dilation
    nb = -(-L // ATT_BLOCK)
    Lp = nb * ATT_BLOCK

    def to_sub(t, front):
        t = jnp.swapaxes(t.reshape(B, L, dilation, H, Dh), 1, 2)
        return jnp.pad(t, ((0, 0), (0, 0), (front, Lp - L), (0, 0), (0, 0)))

    def ctx(t):
        tb = to_sub(t, ATT_BLOCK).reshape(B, dilation, nb + 1, ATT_BLOCK, H, Dh)
        return jnp.concatenate([tb[:, :, :-1], tb[:, :, 1:]], axis=3)

    qb = to_sub(q, 0).reshape(B, dilation, nb, ATT_BLOCK, H, Dh)
    kc, vc = ctx(k), ctx(v)
    s = jnp.einsum('bdnqhe,bdnkhe->bdnhqk', qb, kc,
                   preferred_element_type=jnp.float32) * (Dh ** -0.5)
    qi = jnp.arange(ATT_BLOCK)[:, None] + ATT_BLOCK
    kj = jnp.arange(2 * ATT_BLOCK)[None, :]
    jrel = qi - kj
    kabs = (jnp.arange(nb)[:, None, None] - 1) * ATT_BLOCK + kj[None]
    valid = (jrel >= 0) & (jrel <= n_back) & (kabs >= 0)
    bias = -slopes[:, None, None] * (jrel * dilation).astype(jnp.float32)
    s = jnp.where(valid[None, None, :, None], s + bias[None, None, None], -jnp.inf)
    m = jnp.max(s, -1, keepdims=True)
    p = jnp.exp(s - m)
    den = jnp.sum(p, -1, keepdims=True)
    o = jnp.einsum('bdnhqk,bdnkhe->bdnqhe', p.astype(v.dtype), vc,
                   preferred_element_type=jnp.float32)
    o = o / jnp.swapaxes(den, 3, 4)
    lse = jnp.swapaxes((m + jnp.log(den))[..., 0], 3, 4)

    def from_sub(t):
        t = t.reshape((B, dilation, Lp) + t.shape[4:])[:, :, :L]
        t = jnp.swapaxes(t, 1, 2)
        return t.reshape((B, S) + t.shape[3:])

    return from_sub(o), from_sub(lse)


def dilated_mixture_attention(q, k, v):
    B, S, _ = q.shape
    shp = (B, S, N_DIL_GROUPS, HEADS_PER_GROUP, HEAD_DIM)
    q, k, v = q.reshape(shp), k.reshape(shp), v.reshape(shp)
    slopes = _alibi_slopes()
    outs, lses = [], []
    for g, (window, dilation) in enumerate(DIL_PATTERNS):
        o, l = dilated_group_attention(q[:, :, g], k[:, :, g], v[:, :, g], slopes[g], window, dilation)
        outs.append(o)
        lses.append(l)
    outs = jnp.stack(outs, 0)
    wts = jax.nn.softmax(jnp.stack(lses, 0), axis=0)
    o = jnp.sum(wts[..., None] * outs, axis=0)
    return o.reshape(B, S, DIL_OUT_DIM).astype(q.dtype)


def memory_attention(mq, mk, mv):
    B, S, _ = mq.shape
    q = mq.reshape(B, S, MEM_HEADS, MEM_HEAD_DIM)
    k = mk.reshape(B, -1, MEM_HEADS, MEM_HEAD_DIM)
    v = mv.reshape(B, -1, MEM_HEADS, MEM_HEAD_DIM)
    s = jnp.einsum('bshe,bmhe->bhsm', q, k, preferred_element_type=jnp.float32) * (MEM_HEAD_DIM ** -0.5)
    p = jax.nn.softmax(s, axis=-1)
    o = jnp.einsum('bhsm,bmhe->bshe', p.astype(v.dtype), v)
    return o.reshape(B, S, MEM_DIM)


def hierarchical_moe(h, w_group, b_group, w_router, b_router, w_gate, w_up, w_down):
    glog = (h @ w_group + b_group).astype(jnp.float32)
    gprob = jax.nn.softmax(glog, axis=-1)
    gsel = jnp.argmax(glog, axis=-1)
    gw = jnp.take_along_axis(gprob, gsel[..., None], axis=-1)
    elog_all = jnp.einsum('bsd,gde->bsge', h, w_router) + b_router
    elog = jnp.take_along_axis(elog_all, gsel[..., None, None], axis=2)[:, :, 0].astype(jnp.float32)
    top_v, top_i = lax.top_k(elog, TOP_K)
    top_w = jax.nn.softmax(top_v, axis=-1) * gw
    local = jnp.sum(jax.nn.one_hot(top_i, EXPERTS_PER_GROUP, dtype=jnp.float32) * top_w[..., None], axis=2)
    combine = (jax.nn.one_hot(gsel, N_GROUPS, dtype=jnp.float32)[..., None] * local[:, :, None, :]).astype(h.dtype)
    y = jnp.zeros_like(h)
    for g in range(N_GROUPS):
        a = jnp.einsum('bsd,edf->bsef', h, w_gate[g])
        u = jnp.einsum('bsd,edf->bsef', h, w_up[g])
        hid = jax.nn.silu(a) * u * combine[:, :, g, :, None]
        y = y + jnp.einsum('bsef,efd->bsd', hid, w_down[g])
    return y


def setup_inputs(seed: int = 0) -> dict:
    key = jax.random.key(seed)
    ks = jax.random.split(key, 24)
    f32 = jnp.float32
    D, L = D_MODEL, DEPTH

    def nrm(k, shape, scale):
        return jax.random.normal(k, shape, f32) * scale

    v0 = 3 * CONV_DIM + 2 * DIL_DIM
    col_scale = jnp.ones((IN_DIM,), f32).at[v0:v0 + DIL_DIM].set(BETA)
    mem_col = jnp.ones((2 * MEM_DIM,), f32).at[MEM_DIM:].set(BETA)
    G, E, F = N_GROUPS, EXPERTS_PER_GROUP, D_EXPERT
    return {
        "x": nrm(ks[0], (BATCH, SEQ, D), 1.0),
        "mem": nrm(ks[1], (BATCH, MEM_LEN, D), 1.0),
        "ln_mem_g": 1.0 + nrm(ks[2], (D,), 0.02),
        "ln_mem_b": nrm(ks[3], (D,), 0.02),
        "w_in": nrm(ks[4], (L, D, IN_DIM), D ** -0.5) * col_scale,
        "b_in": nrm(ks[5], (L, IN_DIM), 0.02),
        "w_conv": nrm(ks[6], (L, CONV_WIDTH, CONV_DIM), CONV_WIDTH ** -0.5),
        "w_conv_out": nrm(ks[7], (L, CONV_DIM, D), BETA * CONV_DIM ** -0.5),
        "w_dil_out": nrm(ks[8], (L, DIL_OUT_DIM, D), BETA * DIL_OUT_DIM ** -0.5),
        "w_mem_kv": nrm(ks[9], (L, D, 2 * MEM_DIM), D ** -0.5) * mem_col,
        "w_mem_out": nrm(ks[10], (L, MEM_DIM, D), BETA * MEM_DIM ** -0.5),
        "w_o": nrm(ks[11], (L, D, D), BETA * D ** -0.5),
        "ln1_g": 1.0 + nrm(ks[12], (L, D), 0.02),
        "ln1_b": nrm(ks[13], (L, D), 0.02),
        "w_group": nrm(ks[14], (L, D, G), D ** -0.5),
        "b_group": nrm(ks[15], (L, G), 0.01),
        "w_router": nrm(ks[16], (L, G, D, E), D ** -0.5),
        "b_router": nrm(ks[17], (L, G, E), 0.01),
        "w_gate": nrm(ks[18], (L, G, E, D, F), D ** -0.5),
        "w_up": nrm(ks[19], (L, G, E, D, F), D ** -0.5),
        "w_down": nrm(ks[20], (L, G, E, F, D), BETA * F ** -0.5),
        "ln2_g": 1.0 + nrm(ks[21], (L, D), 0.02),
        "ln2_b": nrm(ks[22], (L, D), 0.02),
    }


def reference(x, mem, ln_mem_g, ln_mem_b, w_in, b_in, w_conv, w_conv_out, w_dil_out,
              w_mem_kv, w_mem_out, w_o, ln1_g, ln1_b, w_group, b_group, w_router, b_router,
              w_gate, w_up, w_down, ln2_g, ln2_b):
    B, S, D = x.shape
    mem_n = layer_norm(mem, ln_mem_g, ln_mem_b)
    splits = _in_splits()
    for l in range(DEPTH):
        proj = x @ w_in[l] + b_in[l]
        cb, cc, ch, q, k, v, mq, gates = jnp.split(proj, splits, axis=-1)
        y_conv = short_conv(cb, cc, ch, w_conv[l]) @ w_conv_out[l]
        y_dil = dilated_mixture_attention(q, k, v) @ w_dil_out[l]
        mk, mv = jnp.split(mem_n @ w_mem_kv[l], 2, axis=-1)
        y_mem = memory_attention(mq, mk, mv) @ w_mem_out[l]
        g = jax.nn.sigmoid(gates).reshape(B, S, N_BRANCHES, D)
        merged = g[:, :, 0] * y_conv + g[:, :, 1] * y_dil + g[:, :, 2] * y_mem
        x = layer_norm(ALPHA * x + merged @ w_o[l], ln1_g[l], ln1_b[l])
        y_moe = hierarchical_moe(x, w_group[l], b_group[l], w_router[l], b_router[l],
                                 w_gate[l], w_up[l], w_down[l])
        x = layer_norm(ALPHA * x + y_moe, ln2_g[l], ln2_b[l])
    return x
```

```python
import numpy as np
import ml_dtypes
from contextlib import ExitStack
import concourse.bass as bass
import concourse.mybir as mybir
from concourse.bass_utils import run_bass_kernel_spmd

F32 = mybir.dt.float32
BF16 = mybir.dt.bfloat16
I32 = mybir.dt.int32
U32 = mybir.dt.uint32
AF = mybir.ActivationFunctionType
ALU = mybir.AluOpType
AX = mybir.AxisListType

LN_EPS = 1e-5
ALIBI_MAX = 8.0

FULL = dict(D=2048, S=2048, CONV=1024, PATTERNS=((128, 1), (512, 4), (2048, 16)), HPG=4,
            ML=256, MEM_HEADS=4, MEM_HD=256, G=4, E=8, F=512, TS=256, DEPTH=1)


def derive(cfg):
    c = dict(cfg)
    c["NG"] = len(c["PATTERNS"])
    c["DIL"] = c["NG"] * c["HPG"] * 128
    c["DIL_OUT"] = c["HPG"] * 128
    c["MEM_DIM"] = c["MEM_HEADS"] * c["MEM_HD"]
    c["IN_DIM"] = 3 * c["CONV"] + 3 * c["DIL"] + c["MEM_DIM"] + 3 * c["D"]
    c["KD"] = c["D"] // 128
    c["NT"] = c["S"] // 128
    c["TGN"] = c["S"] // 512
    c["cB"] = 0
    c["cC"] = c["CONV"]
    c["cH"] = 2 * c["CONV"]
    c["cQ"] = 3 * c["CONV"]
    c["cK"] = c["cQ"] + c["DIL"]
    c["cV"] = c["cK"] + c["DIL"]
    c["cMQ"] = c["cV"] + c["DIL"]
    c["cG"] = c["cMQ"] + c["MEM_DIM"]
    c["NE"] = c["G"] * c["E"]
    c["NR"] = c["G"] + c["NE"]
    c["NTILE"] = (2 * c["S"]) // c["TS"] + c["NE"]
    c["NSLOT"] = c["NTILE"] * c["TS"]
    c["ALPHA"] = (2 * c["DEPTH"]) ** 0.25
    return c


class SemC:
    def __init__(self, h):
        self.h = h
        self.n = 0


class EngH:
    def __init__(self, pb, eng, name):
        self.pb = pb
        self.e = eng
        self.name = name
        self.sc = SemC(pb.newsem("s_" + name))
        self.seen = {}

    def wait(self, toks):
        best = {}
        for t in toks:
            if t is None:
                continue
            sc, v = t
            if self.seen.get(id(sc), 0) >= v:
                continue
            if best.get(id(sc), (None, 0))[1] < v:
                best[id(sc)] = (sc, v)
        for sc, v in best.values():
            self.e.wait_ge(sc.h, v)
            self.seen[id(sc)] = v

    def mark(self, ins):
        ins.then_inc(self.sc.h, 1)
        self.sc.n += 1
        return (self.sc, self.sc.n)


class Dep:
    __slots__ = ("w", "r")

    def __init__(self):
        self.w = None
        self.r = {}


class PB:
    def __init__(self, nc):
        self.nc = nc
        self.es = ExitStack()
        self.deps = {}
        self.nsem = 0
        self.pe = EngH(self, nc.tensor, "pe")
        self.act = EngH(self, nc.scalar, "act")
        self.dve = EngH(self, nc.vector, "dve")
        self.pool = EngH(self, nc.gpsimd, "pool")
        self.sp = EngH(self, nc.sync, "sp")
        self.engs = [self.pe, self.act, self.dve, self.pool, self.sp]
        self.dma_scs = []

    def newsem(self, name):
        self.nsem += 1
        return self.es.enter_context(self.nc.semaphore(name + "_%d" % self.nsem))

    def new_dma_sc(self, name):
        sc = SemC(self.newsem("d_" + name))
        self.dma_scs.append(sc)
        return sc

    def _collect(self, reads, writes):
        toks = []
        for k in reads:
            d = self.deps.get(k)
            if d is not None and d.w is not None:
                toks.append(d.w)
        for k in writes:
            d = self.deps.get(k)
            if d is not None:
                if d.w is not None:
                    toks.append(d.w)
                toks.extend(d.r.values())
        return toks

    def _commit(self, tok, reads, writes):
        for k in reads:
            d = self.deps.setdefault(k, Dep())
            cur = d.r.get(id(tok[0]))
            if cur is None or cur[1] < tok[1]:
                d.r[id(tok[0])] = tok
        for k in writes:
            d = self.deps.setdefault(k, Dep())
            d.w = tok
            d.r = {}

    def op(self, eng, reads, writes, fn, extra=()):
        eng.wait(self._collect(reads, writes) + list(extra))
        tok = eng.mark(fn(eng.e))
        self._commit(tok, reads, writes)
        return tok

    def mm(self, reads, writes, fns):
        eng = self.pe
        eng.wait(self._collect(reads, writes))
        ins = None
        for fn in fns:
            ins = fn(eng.e)
        tok = eng.mark(ins)
        self._commit(tok, reads, writes)
        return tok

    def dma(self, eng, sc, reads, writes, fn):
        eng.wait(self._collect(reads, writes))
        ins = fn(eng.e)
        ins.then_inc(sc.h, 16)
        sc.n += 16
        tok = (sc, sc.n)
        self._commit(tok, reads, writes)
        return tok

    def dma_group(self, eng, sc, reads, writes, fns):
        eng.wait(self._collect(reads, writes))
        for fn in fns:
            ins = fn(eng.e)
            ins.then_inc(sc.h, 16)
            sc.n += 16
        tok = (sc, sc.n)
        self._commit(tok, reads, writes)
        return tok

    def barrier(self):
        toks = [(e.sc, e.sc.n) for e in self.engs if e.sc.n > 0]
        toks += [(sc, sc.n) for sc in self.dma_scs if sc.n > 0]
        for e in self.engs:
            e.wait(toks)
        self.deps = {}


class Ring:
    def __init__(self, n):
        self.n = n
        self.i = -1

    def next(self):
        self.i += 1
        return self.i % self.n


def build_program(cfg, debug=None):
    c = derive(cfg)
    D, S, KD, NT, TGN = c["D"], c["S"], c["KD"], c["NT"], c["TGN"]
    CONV, DIL, DIL_OUT, MEM_DIM, ML = c["CONV"], c["DIL"], c["DIL_OUT"], c["MEM_DIM"], c["ML"]
    HPG, NG, IN_DIM = c["HPG"], c["NG"], c["IN_DIM"]
    NE, NR, F, TS, NTILE, NSLOT = c["NE"], c["NR"], c["F"], c["TS"], c["NTILE"], c["NSLOT"]
    ALPHA = c["ALPHA"]
    NH = NG * HPG
    KC_CONV, KC_DO, KC_MEM = CONV // 128, DIL_OUT // 128, MEM_DIM // 128
    MCH = ML // 128
    HC = c["MEM_HD"] // 128
    FC = F // 128
    SUB = TS // 128

    nc = bass.Bass("TRN2", target_bir_lowering=False)

    def din(name, shape, dt=F32):
        return nc.dram_tensor(name, list(shape), dt, kind="ExternalInput").ap()

    x = din("x", [S, D])
    mem = din("mem", [ML, D])
    lnm_g = din("lnm_g", [128, D])
    lnm_b = din("lnm_b", [128, D])
    w_in = din("w_in", [D, IN_DIM])
    b_col = din("b_col", [128, IN_DIM // 128])
    wconv_col = din("wconv_col", [128, 3, CONV // 128])
    w_conv_out = din("w_conv_out", [CONV, D])
    w_dil_out = din("w_dil_out", [DIL_OUT, D])
    w_mem_kv = din("w_mem_kv", [D, 2 * MEM_DIM])
    w_mem_out = din("w_mem_out", [MEM_DIM, D])
    w_o = din("w_o", [D, D])
    ln1_g = din("ln1_g", [128, D])
    ln1_b = din("ln1_b", [128, D])
    w_r = din("w_r", [D, NR])
    b_r = din("b_r", [128, NR])
    w_gate = din("w_gate", [NE * 128, KD * F])
    w_up = din("w_up", [NE * 128, KD * F])
    w_down = din("w_down", [NE * 128, (F // 128) * D])
    iota_p = din("iota_p", [128, 1])
    ln2_g = din("ln2_g", [128, D])
    ln2_b = din("ln2_b", [128, D])
    ident_f = din("ident_f", [128, 128])
    ident_b = din("ident_b", [128, 128], BF16)
    ebt = din("ebt", [NH, 128, 2, 128])
    tri = din("tri", [128, 128])
    iota_e = din("iota_e", [128, NE])
    iota_t = din("iota_t", [128, NTILE])

    out = nc.dram_tensor("out", [S, D], F32, kind="ExternalOutput").ap()
    mergedT = nc.dram_tensor("mergedT", [KD, 128, S], BF16,
                             kind="ExternalOutput" if debug == "merged" else "Internal").ap()

    X1F = nc.dram_tensor("X1F", [S, D], F32, kind="ExternalOutput" if debug == "x1" else "Internal").ap()
    X1B = nc.dram_tensor("X1B", [S, D], BF16, kind="Internal").ap()
    Xs = nc.dram_tensor("Xs", [NSLOT, D], BF16, kind="ExternalOutput" if debug == "route" else "Internal").ap()
    Ys = nc.dram_tensor("Ys", [NSLOT, D], F32, kind="Internal").ap()

    pb = PB(nc)
    pe, act, dve, pool, sp = pb.pe, pb.act, pb.dve, pb.pool, pb.sp

    with pb.es:
        top = pb.es

        def sbuf(stack, name, shape, dt):
            return stack.enter_context(nc.sbuf_tensor(name, list(shape), dt))

        psum = top.enter_context(nc.psum_tensor("psum", [128, 8, 512], F32))
        ps_ring = Ring(8)

        def ps_next():
            i = ps_ring.next()
            return i, ("ps", i)

        cst_sc = pb.new_dma_sc("cst")
        idf = sbuf(top, "idf", [128, 128], F32)
        idb = sbuf(top, "idb", [128, 128], BF16)
        ones_b = sbuf(top, "ones_b", [128, 128], BF16)
        bcol = sbuf(top, "bcol", [128, IN_DIM // 128], F32)
        wcc = sbuf(top, "wcc", [128, 3, CONV // 128], F32)
        for (dst, src, key) in ((idf, ident_f, "idf"), (idb, ident_b, "idb"), (bcol, b_col, "bcol"),
                                (wcc, wconv_col, "wcc")):
            pb.dma(sp, cst_sc, [], [key], lambda e, dst=dst, src=src: e.dma_start(out=dst[:], in_=src))
        pb.op(dve, [], ["ones_b"], lambda e: e.memset(ones_b[:], 1.0))
        zt = sbuf(top, "zt", [128, D], BF16)
        pb.op(dve, [], ["zt"], lambda e: e.memset(zt[:], 0.0))
        zf_sc = SemC(pb.newsem("d_zf"))
        NZ = NSLOT // 128
        ZG = 8
        zf_box = []

        def emit_zero_fill():
            zf_box.append(pb.dma_group(sp, zf_sc, ["zt"], [],
                          [lambda e, z0=z0: e.dma_start(
                              out=Xs[z0 * 128:min(NZ, z0 + ZG) * 128, :].rearrange("(n p) d -> p n d", p=128),
                              in_=zt[:].unsqueeze(1).to_broadcast([128, min(NZ, z0 + ZG) - z0, D]))
                           for z0 in range(0, NZ, ZG)]))

        WR = 6
        wbuf = []
        wsc = [pb.new_dma_sc("w%d" % i) for i in range(WR)]
        wring = Ring(WR)

        def wload(src, kc_n):
            s = wring.next()
            pb.dma(pool, wsc[s], [], [("wb", s)],
                   lambda e: e.dma_start(out=wbuf[s][:, 0:kc_n, :],
                                         in_=src.rearrange("(kc p) n -> p kc n", p=128)))
            return s

        def proj_fm(src, kc_n, rhs_fn, rhs_keys, n_groups, consume, n_cols=512, out_fn=None):
            s = wload(src, kc_n)
            for g in range(n_groups):
                bi, bk = ps_next()
                o_ap = psum[:, bi, 0:n_cols] if out_fn is None else out_fn(psum[:, bi, 0:n_cols])
                pb.mm([("wb", s)] + list(rhs_keys), [bk],
                      [lambda e, kc=kc, g=g, bi=bi: e.matmul(
                          o_ap, wbuf[s][:, kc, :], rhs_fn(kc, g),
                          start=(kc == 0), stop=(kc == kc_n - 1)) for kc in range(kc_n)])
                consume(g, bi, bk)

        with ExitStack() as s1:
            wbuf.extend(sbuf(s1, "wb%d" % i, [128, KD, 128], BF16) for i in range(WR))
            xT = sbuf(s1, "xT", [128, KD, S], BF16)
            mkT = sbuf(s1, "mkT", [128, KC_MEM, ML], BF16)
            mvt = sbuf(s1, "mvt", [128, MCH, MEM_DIM], BF16)

            with ExitStack() as p0:
                memx = sbuf(p0, "memx", [128, MCH, D], F32)
                memn = sbuf(p0, "memn", [128, MCH, D], BF16)
                memnT = sbuf(p0, "memnT", [128, KD, ML], BF16)
                lng = sbuf(p0, "lng", [128, D], F32)
                lnb = sbuf(p0, "lnb", [128, D], F32)
                xn = sbuf(p0, "xn", [128, D], F32)
                nchunk = max(1, D // 512)
                cw = D // nchunk
                st6 = sbuf(p0, "st6", [128, nchunk, 6], F32)
                mv2 = sbuf(p0, "mv2", [128, 4], F32)
                xs = [sbuf(p0, "xs%d" % i, [128, D], F32) for i in range(2)]
                xs_sc = [pb.new_dma_sc("xs%d" % i) for i in range(2)]
                pb.dma(sp, cst_sc, [], ["memx"],
                       lambda e: e.dma_start(out=memx[:], in_=mem.rearrange("(t p) d -> p t d", p=128)))
                pb.dma(sp, cst_sc, [], ["lng"], lambda e: e.dma_start(out=lng[:], in_=lnm_g))
                pb.dma(sp, cst_sc, [], ["lnb"], lambda e: e.dma_start(out=lnb[:], in_=lnm_b))
                cst_tok = (cst_sc, cst_sc.n)
                for k in ("idf", "idb", "bcol", "wcc", "memx", "lng", "lnb"):
                    pb.deps[k].w = cst_tok

                for t in range(MCH):
                    for ci in range(nchunk):
                        pb.op(dve, ["memx"], [("st6", ci)],
                              lambda e, ci=ci: e.bn_stats(out=st6[:, ci, :], in_=memx[:, t, ci * cw:(ci + 1) * cw]))
                    pb.op(dve, [("st6", ci) for ci in range(nchunk)], ["mv2"],
                          lambda e: e.bn_aggr(out=mv2[:, 0:2], in_=st6[:]))
                    pb.op(dve, ["mv2"], ["mv2r"],
                          lambda e: e.tensor_scalar(out=mv2[:, 2:3], in0=mv2[:, 1:2], scalar1=LN_EPS, scalar2=None,
                                                    op0=ALU.add))
                    pb.op(act, ["mv2r"], ["mv2r"],
                          lambda e: e.activation(out=mv2[:, 2:3], in_=mv2[:, 2:3], func=AF.Sqrt))
                    pb.op(dve, ["mv2r"], ["mv2r"], lambda e: e.reciprocal(out=mv2[:, 2:3], in_=mv2[:, 2:3]))
                    pb.op(dve, ["mv2", "mv2r"], ["mv2n"],
                          lambda e: e.tensor_scalar(out=mv2[:, 3:4], in0=mv2[:, 0:1], scalar1=mv2[:, 2:3], scalar2=-1.0,
                                                    op0=ALU.mult, op1=ALU.mult))
                    pb.op(act, ["memx", "mv2r", "mv2n"], ["xn"],
                          lambda e: e.activation(out=xn[:], in_=memx[:, t, :], func=AF.Identity,
                                                 scale=mv2[:, 2:3], bias=mv2[:, 3:4]))
                    pb.op(dve, ["xn", "lng"], ["xn"], lambda e: e.tensor_tensor(out=xn[:], in0=xn[:], in1=lng[:], op=ALU.mult))
                    pb.op(dve, ["xn", "lnb"], [("memn", t)],
                          lambda e: e.tensor_tensor(out=memn[:, t, :], in0=xn[:], in1=lnb[:], op=ALU.add))
                    for k0 in range(0, KD, 8):
                        kn = min(8, KD - k0)
                        bi, bk = ps_next()
                        pbf = psum[:, bi, :].bitcast(BF16)
                        pb.mm([("memn", t), "idb"], [bk],
                              [lambda e, kk=kk, bi=bi, pbf=pbf: e.transpose(
                                  pbf[:, (kk - k0) * 128:(kk - k0 + 1) * 128], memn[:, t, kk * 128:(kk + 1) * 128], idb[:])
                               for kk in range(k0, k0 + kn)])
                        pb.op(act, [bk], [("memnT", t, k0)],
                              lambda e, pbf=pbf, kn=kn, k0=k0: e.activation(
                                  out=memnT[:, k0:k0 + kn, t * 128:(t + 1) * 128],
                                  in_=pbf[:, 0:kn * 128].rearrange("p (a b) -> p a b", a=kn), func=AF.Copy))
                memnT_keys = [("memnT", t, k0) for t in range(MCH) for k0 in range(0, KD, 8)]
                for cc_ in range(KC_MEM):
                    def cons(g, bi, bk, cc_=cc_):
                        pb.op(act, [bk], [("mkT", cc_)],
                              lambda e: e.activation(out=mkT[:, cc_, :], in_=psum[:, bi, 0:ML], func=AF.Copy))
                    proj_fm(w_mem_kv[:, cc_ * 128:(cc_ + 1) * 128], KD, lambda kc, g: memnT[:, kc, :],
                            memnT_keys, 1, cons, n_cols=ML)
                for cc_ in range(KC_MEM):
                    s = wload(w_mem_kv[:, MEM_DIM + cc_ * 128: MEM_DIM + (cc_ + 1) * 128], KD)
                    for mc in range(MCH):
                        bi, bk = ps_next()
                        pb.mm([("wb", s)] + memnT_keys, [bk],
                              [lambda e, kc=kc, bi=bi, mc=mc: e.matmul(
                                  psum[:, bi, 0:128], memnT[:, kc, mc * 128:(mc + 1) * 128], wbuf[s][:, kc, :],
                                  start=(kc == 0), stop=(kc == KD - 1)) for kc in range(KD)])
                        pb.op(act, [bk], [("mvt", mc, cc_)],
                              lambda e, bi=bi, mc=mc, cc_=cc_: e.activation(
                                  out=mvt[:, mc, cc_ * 128:(cc_ + 1) * 128], in_=psum[:, bi, 0:128], func=AF.Copy))

                for tt in range(NT):
                    b = tt % 2
                    pb.dma(sp, xs_sc[b], [], [("xs", b)],
                           lambda e, b=b, tt=tt: e.dma_start(out=xs[b][:], in_=x[tt * 128:(tt + 1) * 128, :]))
                    for k0 in range(0, KD, 4):
                        kn = min(4, KD - k0)
                        bi, bk = ps_next()
                        pb.mm([("xs", b), "idf"], [bk],
                              [lambda e, kk=kk, bi=bi, b=b: e.transpose(
                                  psum[:, bi, (kk - k0) * 128:(kk - k0 + 1) * 128], xs[b][:, kk * 128:(kk + 1) * 128], idf[:])
                               for kk in range(k0, k0 + kn)])
                        ev = act if (k0 // 4) % 2 == 0 else dve
                        if ev is act:
                            pb.op(act, [bk], [("xT", tt, k0)],
                                  lambda e, bi=bi, kn=kn, k0=k0, tt=tt: e.activation(
                                      out=xT[:, k0:k0 + kn, tt * 128:(tt + 1) * 128],
                                      in_=psum[:, bi, 0:kn * 128].rearrange("p (a b) -> p a b", a=kn), func=AF.Copy))
                        else:
                            pb.op(dve, [bk], [("xT", tt, k0)],
                                  lambda e, bi=bi, kn=kn, k0=k0, tt=tt: e.tensor_copy(
                                      out=xT[:, k0:k0 + kn, tt * 128:(tt + 1) * 128],
                                      in_=psum[:, bi, 0:kn * 128].rearrange("p (a b) -> p a b", a=kn)))
                emit_zero_fill()
                pb.barrier()
            xT_keys = ["xTall"]
            pb.deps["xTall"] = Dep()

            def x_nat(kc, g):
                return xT[:, kc, g * 512:(g + 1) * 512]

            convT = sbuf(s1, "convT", [128, KC_CONV, S], BF16)
            with ExitStack() as p2:
                cct = [sbuf(p2, "cct%d" % i, [128, 512], F32) for i in range(2)]
                ub = [sbuf(p2, "ub%d" % i, [128, S + 2], F32) for i in range(2)]
                yb = [sbuf(p2, "yb%d" % i, [128, S], F32) for i in range(2)]
                for i in range(2):
                    pb.op(dve, [], [("u", i)], lambda e, i=i: e.memset(ub[i][:, 0:2], 0.0))
                cct_ring = Ring(2)
                for f in range(KC_CONV):
                    ui = f % 2
                    colC = (c["cC"] // 128) + f
                    colH = (c["cH"] // 128) + f
                    colB = (c["cB"] // 128) + f
                    slots = {}

                    def cons_c(g, bi, bk):
                        ci = cct_ring.next()
                        slots[g] = ci
                        pb.op(act, [bk, "bcol"], [("cct", ci)],
                              lambda e: e.activation(out=cct[ci][:], in_=psum[:, bi, :], func=AF.Identity,
                                                     bias=bcol[:, colC:colC + 1]))

                    def cons_h(g, bi, bk):
                        ci = slots[g]
                        pb.op(dve, [bk, "bcol", ("cct", ci)], [("u", ui)],
                              lambda e: e.scalar_tensor_tensor(
                                  out=ub[ui][:, 2 + g * 512: 2 + (g + 1) * 512], in0=psum[:, bi, :],
                                  scalar=bcol[:, colH:colH + 1], in1=cct[ci][:], op0=ALU.add, op1=ALU.mult))

                    sC = wload(w_in[:, colC * 128:(colC + 1) * 128], KD)
                    sH = wload(w_in[:, colH * 128:(colH + 1) * 128], KD)
                    for g in range(TGN):
                        for (s, cons) in ((sC, cons_c), (sH, cons_h)):
                            bi, bk = ps_next()
                            pb.mm([("wb", s)] + xT_keys, [bk],
                                  [lambda e, kc=kc, bi=bi, s=s, g=g: e.matmul(
                                      psum[:, bi, :], wbuf[s][:, kc, :], x_nat(kc, g),
                                      start=(kc == 0), stop=(kc == KD - 1)) for kc in range(KD)])
                            cons(g, bi, bk)
                    pb.op(act, [("u", ui), "wcc"], [("y", ui)],
                          lambda e: e.activation(out=yb[ui][:], in_=ub[ui][:, 2:S + 2], func=AF.Copy,
                                                 scale=wcc[:, 2, f:f + 1]))
                    pb.op(dve, [("u", ui), "wcc", ("y", ui)], [("y", ui)],
                          lambda e: e.scalar_tensor_tensor(out=yb[ui][:], in0=ub[ui][:, 1:S + 1], scalar=wcc[:, 1, f:f + 1],
                                                           in1=yb[ui][:], op0=ALU.mult, op1=ALU.add))
                    pb.op(dve, [("u", ui), "wcc", ("y", ui)], [("y", ui)],
                          lambda e: e.scalar_tensor_tensor(out=yb[ui][:], in0=ub[ui][:, 0:S], scalar=wcc[:, 0, f:f + 1],
                                                           in1=yb[ui][:], op0=ALU.mult, op1=ALU.add))

                    def cons_b(g, bi, bk):
                        pb.op(dve, [bk, "bcol", ("y", ui)], [("convT", f, g)],
                              lambda e: e.scalar_tensor_tensor(
                                  out=convT[:, f, g * 512:(g + 1) * 512], in0=psum[:, bi, :],
                                  scalar=bcol[:, colB:colB + 1], in1=yb[ui][:, g * 512:(g + 1) * 512],
                                  op0=ALU.add, op1=ALU.mult))
                    proj_fm(w_in[:, colB * 128:(colB + 1) * 128], KD, x_nat, xT_keys, TGN, cons_b)
                pb.barrier()

            def dbg_dump(t, kcn):
                dbg = nc.dram_tensor("dbg", [kcn, 128, S], BF16, kind="ExternalOutput").ap()
                dsc = pb.new_dma_sc("dbg")
                pb.barrier()
                pb.dma(sp, dsc, [], [], lambda e: e.dma_start(out=dbg.rearrange("k p s -> p k s"), in_=t[:]))
                sp.wait([(dsc, dsc.n)])

            if debug == "conv":
                dbg_dump(convT, KC_CONV)
                return nc, c

            att_scale = 128.0 ** -0.5
            dilT = sbuf(s1, "dilT", [128, KC_DO, S], BF16)
            with ExitStack() as p3:
                qT = [sbuf(p3, "qT%d" % i, [128, S], BF16) for i in range(1)]
                kT = [sbuf(p3, "kT%d" % i, [128, S], BF16) for i in range(1)]
                vT = [sbuf(p3, "vT%d" % i, [128, S], BF16) for i in range(1)]
                Vt = [sbuf(p3, "Vt%d" % i, [128, NT, 128], BF16) for i in range(1)]
                ebs = [sbuf(p3, "ebs%d" % i, [128, 2, 128], F32) for i in range(2)]
                eb_sc = [pb.new_dma_sc("eb%d" % i) for i in range(2)]
                NEB = 3
                Eb = [sbuf(p3, "Eb%d" % i, [128, 2, 128], F32) for i in range(NEB)]
                PT = [sbuf(p3, "PT%d" % i, [128, 2, 128], BF16) for i in range(NEB)]
                e_ring = Ring(NEB)
                OD = sbuf(p3, "OD", [128, 2, S], F32)
                hcount = 0
                for hh in range(HPG):
                    for g in range(NG):
                        window, d = c["PATTERNS"][g]
                        L = S // d
                        assert L % 128 == 0
                        nblk = L // 128
                        h = g * HPG + hh
                        hb = 0
                        ebi = hcount % 2
                        hcount += 1
                        pb.dma(sp, eb_sc[ebi], [], [("eb", ebi)], lambda e: e.dma_start(out=ebs[ebi][:], in_=ebt[h]))

                        def x_perm(kc, tg):
                            view = xT[:, kc, :].rearrange("p (i r) -> p r i", r=d)
                            if L >= 512:
                                r = (tg * 512) // L
                                i0 = (tg * 512) % L
                                return view[:, r, i0:i0 + 512]
                            nr = 512 // L
                            return view[:, tg * nr:(tg + 1) * nr, :]

                        ni = 512 // d
                        for (nm, colbase, dst) in (("q", c["cQ"], qT[hb]), ("k", c["cK"], kT[hb]), ("v", c["cV"], vT[hb])):
                            col = (colbase // 128) + g * HPG + hh

                            def cons(tg, bi, bk):
                                if d == 1:
                                    o_ap = dst[:, tg * 512:(tg + 1) * 512]
                                    i_ap = psum[:, bi, :]
                                else:
                                    o_ap = dst[:, :].rearrange("p (r i) -> p i r", r=d)[:, tg * ni:(tg + 1) * ni, :]
                                    i_ap = psum[:, bi, :].rearrange("p (i r) -> p i r", r=d)
                                pb.op(act, [bk, "bcol"], [(nm, hb)],
                                      lambda e: e.activation(out=o_ap, in_=i_ap, func=AF.Identity, bias=bcol[:, col:col + 1]))
                            proj_fm(w_in[:, col * 128:(col + 1) * 128], KD, x_nat, xT_keys, TGN, cons)
                        for b0 in range(0, NT, 8):
                            bn = min(8, NT - b0)
                            bi, bk = ps_next()
                            pbf = psum[:, bi, :].bitcast(BF16)
                            pb.mm([("v", hb), "idb"], [bk],
                                  [lambda e, bb=bb: e.transpose(pbf[:, (bb - b0) * 128:(bb - b0 + 1) * 128],
                                                                vT[hb][:, bb * 128:(bb + 1) * 128], idb[:])
                                   for bb in range(b0, b0 + bn)])
                            pb.op(dve, [bk], [("Vt", hb, b0)],
                                  lambda e: e.tensor_copy(out=Vt[hb][:, b0:b0 + bn, :],
                                                          in_=pbf[:, 0:bn * 128].rearrange("p (a b) -> p a b", a=bn)))
                        qk_keys = [("q", hb), ("k", hb)]
                        vt_keys = [("Vt", hb, b0) for b0 in range(0, NT, 8)]

                        def emit_s(bidx):
                            n = bidx % nblk
                            nk = 2 if n > 0 else 1
                            pbase = bidx * 128
                            bi, bk = ps_next()
                            fns = [lambda e: e.matmul(psum[:, bi, 0:128], kT[hb][:, pbase:pbase + 128],
                                                      qT[hb][:, pbase:pbase + 128], start=True, stop=True)]
                            if nk == 2:
                                fns.append(lambda e: e.matmul(psum[:, bi, 128:256], kT[hb][:, pbase - 128:pbase],
                                                              qT[hb][:, pbase:pbase + 128], start=True, stop=True))
                            pb.mm(qk_keys, [bk], fns)
                            ei = e_ring.next()
                            pb.op(act, [bk], [("Eb", ei)],
                                  lambda e: e.activation(out=Eb[ei][:, 0:nk, :],
                                                         in_=psum[:, bi, 0:nk * 128].rearrange("p (a b) -> p a b", a=nk),
                                                         func=AF.Exp, scale=att_scale))
                            pb.op(dve, [("Eb", ei), ("eb", ebi)], [("PT", ei)],
                                  lambda e: e.tensor_tensor(out=PT[ei][:, 0:nk, :], in0=Eb[ei][:, 0:nk, :],
                                                            in1=ebs[ebi][:, 0:nk, :], op=ALU.mult))
                            return (bidx, nk, ei)

                        def emit_pv(st):
                            bidx, nk, ei = st
                            r = bidx // nblk
                            n = bidx % nblk
                            bo, bko = ps_next()
                            fns = [lambda e: e.matmul(psum[:, bo, 0:128], Vt[hb][:, bidx, :], PT[ei][:, 0, :],
                                                      start=True, stop=(nk == 1))]
                            if nk == 2:
                                fns.append(lambda e: e.matmul(psum[:, bo, 0:128], Vt[hb][:, bidx - 1, :], PT[ei][:, 1, :],
                                                              start=False, stop=True))
                            fns.append(lambda e: e.matmul(psum[:, bo, 128:256], ones_b[:], PT[ei][:, 0, :],
                                                          start=True, stop=(nk == 1)))
                            if nk == 2:
                                fns.append(lambda e: e.matmul(psum[:, bo, 128:256], ones_b[:], PT[ei][:, 1, :],
                                                              start=False, stop=True))
                            pb.mm(vt_keys + [("PT", ei), "ones_b"], [bko], fns)
                            t0 = r + d * n * 128
                            od_ap = OD[:, :, t0:min(S, t0 + d * 128):d]
                            src = psum[:, bo, 0:256].rearrange("p (a b) -> p a b", a=2)
                            if g == 0:
                                pb.op(act, [bko], ["OD"], lambda e: e.activation(out=od_ap, in_=src, func=AF.Copy))
                            else:
                                pb.op(dve, [bko, "OD"], ["OD"],
                                      lambda e: e.tensor_tensor(out=od_ap, in0=src, in1=od_ap, op=ALU.add))

                        pend = None
                        for bidx in range(NT):
                            st = emit_s(bidx)
                            if pend is not None:
                                emit_pv(pend)
                            pend = st
                        emit_pv(pend)
                    pb.op(dve, ["OD"], ["OD"], lambda e: e.reciprocal(out=OD[:, 1, :], in_=OD[:, 1, :]))
                    pb.op(dve, ["OD"], [("dilT", hh)],
                          lambda e: e.tensor_tensor(out=dilT[:, hh, :], in0=OD[:, 0, :], in1=OD[:, 1, :], op=ALU.mult))
                pb.barrier()

            if debug == "dil":
                dbg_dump(dilT, KC_DO)
                return nc, c

            mem_scale = float(c["MEM_HD"]) ** -0.5
            memT = sbuf(s1, "memT", [128, KC_MEM, S], BF16)
            with ExitStack() as p4:
                mqT = [sbuf(p4, "mqT%d" % i, [128, HC, S], BF16) for i in range(2)]
                PTm = [sbuf(p4, "PTm%d" % i, [128, MCH, 512], BF16) for i in range(2)]
                rDm = [sbuf(p4, "rDm%d" % i, [128, 512], F32) for i in range(2)]
                pt_ring = Ring(2)
                for mh in range(c["MEM_HEADS"]):
                    hb = mh % 2
                    for hc in range(HC):
                        col = (c["cMQ"] // 128) + mh * HC + hc

                        def cons(tg, bi, bk):
                            pb.op(act, [bk, "bcol"], [("mq", hb, hc, tg)],
                                  lambda e: e.activation(out=mqT[hb][:, hc, tg * 512:(tg + 1) * 512], in_=psum[:, bi, :],
                                                         func=AF.Identity, bias=bcol[:, col:col + 1]))
                        proj_fm(w_in[:, col * 128:(col + 1) * 128], KD, x_nat, xT_keys, TGN, cons)
                    for tg in range(TGN):
                        pi = pt_ring.next()
                        for mc in range(MCH):
                            bi, bk = ps_next()
                            pb.mm([("mq", hb, hc, tg) for hc in range(HC)], [bk],
                                  [lambda e, hc=hc: e.matmul(psum[:, bi, :], mkT[:, mh * HC + hc, mc * 128:(mc + 1) * 128],
                                                             mqT[hb][:, hc, tg * 512:(tg + 1) * 512],
                                                             start=(hc == 0), stop=(hc == HC - 1)) for hc in range(HC)])
                            pb.op(act, [bk], [("PTm", pi, mc)],
                                  lambda e: e.activation(out=PTm[pi][:, mc, :], in_=psum[:, bi, :], func=AF.Exp,
                                                         scale=mem_scale))
                        ptk = [("PTm", pi, mc) for mc in range(MCH)]
                        bd, bkd = ps_next()
                        pb.mm(ptk + ["ones_b"], [bkd],
                              [lambda e, mc=mc: e.matmul(psum[:, bd, :], ones_b[:], PTm[pi][:, mc, :],
                                                         start=(mc == 0), stop=(mc == MCH - 1)) for mc in range(MCH)])
                        pb.op(dve, [bkd], [("rDm", pi)], lambda e: e.reciprocal(out=rDm[pi][:], in_=psum[:, bd, :]))
                        for oc in range(HC):
                            bo, bko = ps_next()
                            pb.mm(ptk, [bko],
                                  [lambda e, mc=mc: e.matmul(psum[:, bo, :],
                                                             mvt[:, mc, mh * c["MEM_HD"] + oc * 128: mh * c["MEM_HD"] + (oc + 1) * 128],
                                                             PTm[pi][:, mc, :], start=(mc == 0), stop=(mc == MCH - 1))
                                   for mc in range(MCH)])
                            pb.op(dve, [bko, ("rDm", pi)], [("memT", mh * HC + oc, tg)],
                                  lambda e: e.tensor_tensor(out=memT[:, mh * HC + oc, tg * 512:(tg + 1) * 512],
                                                            in0=psum[:, bo, :], in1=rDm[pi][:], op=ALU.mult))
                pb.barrier()

            if debug == "mem":
                dbg_dump(memT, KC_MEM)
                return nc, c

            with ExitStack() as p5:
                sg = [sbuf(p5, "sg%d" % i, [128, S], F32) for i in range(1)]
                macc = [sbuf(p5, "macc%d" % i, [128, S], F32) for i in range(1)]
                tmpm = [sbuf(p5, "tmpm%d" % i, [128, 512], F32) for i in range(2)]
                mrg = [sbuf(p5, "mrg%d" % i, [128, S], BF16) for i in range(1)]
                mrg_sc = [pb.new_dma_sc("mrg%d" % i) for i in range(1)]
                tm_ring = Ring(2)
                branches = ((w_conv_out, KC_CONV, convT), (w_dil_out, KC_DO, dilT), (w_mem_out, KC_MEM, memT))
                for j in range(KD):
                    jb = 0
                    for br, (wout, kcn, actT) in enumerate(branches):
                        gcol = (c["cG"] // 128) + br * KD + j

                        def cons_g(tg, bi, bk):
                            pb.op(act, [bk, "bcol"], [("sg", jb, tg)],
                                  lambda e: e.activation(out=sg[jb][:, tg * 512:(tg + 1) * 512], in_=psum[:, bi, :],
                                                         func=AF.Sigmoid, bias=bcol[:, gcol:gcol + 1]))
                        proj_fm(w_in[:, gcol * 128:(gcol + 1) * 128], KD, x_nat, xT_keys, TGN, cons_g)

                        def cons_y(tg, bi, bk):
                            sl = slice(tg * 512, (tg + 1) * 512)
                            if br == 0:
                                pb.op(dve, [bk, ("sg", jb, tg)], [("macc", jb, tg)],
                                      lambda e: e.tensor_tensor(out=macc[jb][:, sl], in0=psum[:, bi, :], in1=sg[jb][:, sl],
                                                                op=ALU.mult))
                            else:
                                ti = tm_ring.next()
                                pb.op(dve, [bk, ("sg", jb, tg)], [("tmpm", ti)],
                                      lambda e: e.tensor_tensor(out=tmpm[ti][:], in0=psum[:, bi, :], in1=sg[jb][:, sl],
                                                                op=ALU.mult))
                                if br == 1:
                                    pb.op(dve, [("tmpm", ti), ("macc", jb, tg)], [("macc", jb, tg)],
                                          lambda e: e.tensor_tensor(out=macc[jb][:, sl], in0=tmpm[ti][:], in1=macc[jb][:, sl],
                                                                    op=ALU.add))
                                else:
                                    pb.op(dve, [("tmpm", ti), ("macc", jb, tg)], [("mrg", jb)],
                                          lambda e: e.tensor_tensor(out=mrg[jb][:, sl], in0=tmpm[ti][:], in1=macc[jb][:, sl],
                                                                    op=ALU.add))
                        proj_fm(wout[:, j * 128:(j + 1) * 128], kcn, lambda kc, tg: actT[:, kc, tg * 512:(tg + 1) * 512],
                                [], TGN, cons_y)
                    pb.dma(sp, mrg_sc[jb], [("mrg", jb)], [("mergedT", j)],
                           lambda e: e.dma_start(out=mergedT[j], in_=mrg[jb][:]))
                pb.barrier()

            if debug == "merged":
                sp.wait([(sc_, sc_.n) for sc_ in mrg_sc])
                return nc, c

        rt = ExitStack()
        top.enter_context(rt)
        ones_f = sbuf(rt, "ones_f", [128, 128], F32)
        trif = sbuf(rt, "trif", [128, 128], F32)
        OH1 = sbuf(rt, "OH1", [128, NT, NE], F32)
        OH2 = sbuf(rt, "OH2", [128, NT, NE], F32)
        OHs = sbuf(rt, "OHs", [128, NT, NE], F32)
        w12 = sbuf(rt, "w12", [128, NT, 2], F32)
        slf = sbuf(rt, "slf", [128, NT, 2], F32)
        sli = sbuf(rt, "sli", [128, NT, 2], I32)
        soff = sbuf(rt, "soff", [128, NE], F32)
        toff = sbuf(rt, "toff", [128, NE], F32)
        tei = sbuf(rt, "tei", [128, NTILE], I32)
        iop = sbuf(rt, "iop", [128, 1], F32)
        gln = sbuf(rt, "gln", [128, D], F32)
        bln = sbuf(rt, "bln", [128, D], F32)
        pb.op(dve, [], ["ones_f"], lambda e: e.memset(ones_f[:], 1.0))
        c2_sc = pb.new_dma_sc("c2")
        pb.dma(sp, c2_sc, [], ["trif"], lambda e: e.dma_start(out=trif[:], in_=tri))
        pb.dma(sp, c2_sc, [], ["iop"], lambda e: e.dma_start(out=iop[:], in_=iota_p))

        def ln_rows(src_ap, dst_ap, src_key, dst_key, st6, mv4, g_ap, b_ap, gb_keys, mul_eng=None):
            mul_eng = mul_eng or pool
            nch = max(1, D // 512)
            cw_ = D // nch
            for ci in range(nch):
                pb.op(dve, [src_key], [("st6", ci)],
                      lambda e: e.bn_stats(out=st6[:, ci, :], in_=src_ap[:, ci * cw_:(ci + 1) * cw_]))
            pb.op(dve, [("st6", ci) for ci in range(nch)], ["mv4"], lambda e: e.bn_aggr(out=mv4[:, 0:2], in_=st6[:]))
            pb.op(dve, ["mv4"], ["mv4r"],
                  lambda e: e.tensor_scalar(out=mv4[:, 2:3], in0=mv4[:, 1:2], scalar1=LN_EPS, scalar2=None, op0=ALU.add))
            pb.op(act, ["mv4r"], ["mv4r"], lambda e: e.activation(out=mv4[:, 2:3], in_=mv4[:, 2:3], func=AF.Ln))
            pb.op(act, ["mv4r"], ["mv4r"], lambda e: e.activation(out=mv4[:, 2:3], in_=mv4[:, 2:3], func=AF.Exp, scale=-0.5))
            pb.op(dve, ["mv4", "mv4r"], ["mv4n"],
                  lambda e: e.tensor_scalar(out=mv4[:, 3:4], in0=mv4[:, 0:1], scalar1=mv4[:, 2:3], scalar2=-1.0,
                                            op0=ALU.mult, op1=ALU.mult))
            pb.op(act, [src_key, "mv4r", "mv4n"], [src_key],
                  lambda e: e.activation(out=src_ap, in_=src_ap, func=AF.Identity, scale=mv4[:, 2:3], bias=mv4[:, 3:4]))
            pb.op(mul_eng, [src_key] + gb_keys, [src_key],
                  lambda e: e.tensor_tensor(out=src_ap, in0=src_ap, in1=g_ap, op=ALU.mult))
            pb.op(dve, [src_key] + gb_keys, [dst_key],
                  lambda e: e.tensor_tensor(out=dst_ap, in0=src_ap, in1=b_ap, op=ALU.add))

        with ExitStack() as p6:
            wo = sbuf(p6, "wo", [128, KD, D], BF16)
            wo_sc = pb.new_dma_sc("wo")
            NQ = max(1, D // 512)
            qw = D // NQ
            for n in range(NQ):
                pb.dma(pool, wo_sc, [], [("wo", n)],
                       lambda e: e.dma_start(out=wo[:, :, n * qw:(n + 1) * qw],
                                             in_=w_o[:, n * qw:(n + 1) * qw].rearrange("(kc p) n -> p kc n", p=128)))
            wo_tok = (wo_sc, wo_sc.n)
            for n in range(NQ):
                pb.deps[("wo", n)].w = wo_tok
            pb.dma(sp, c2_sc, [], ["gln"], lambda e: e.dma_start(out=gln[:], in_=ln1_g))
            pb.dma(sp, c2_sc, [], ["bln"], lambda e: e.dma_start(out=bln[:], in_=ln1_b))
            wr = sbuf(p6, "wr", [128, KD, NR], F32)
            brt = sbuf(p6, "brt", [128, NR], F32)
            pb.dma(sp, c2_sc, [], ["wr"], lambda e: e.dma_start(out=wr[:], in_=w_r.rearrange("(kc p) n -> p kc n", p=128)))
            pb.dma(sp, c2_sc, [], ["brt"], lambda e: e.dma_start(out=brt[:], in_=b_r))
            c2_tok = (c2_sc, c2_sc.n)
            for k in ("trif", "iop", "gln", "bln", "wr", "brt"):
                pb.deps[k].w = c2_tok
            GT = min(2, NT)
            mTg = [sbuf(p6, "mTg%d" % i, [128, KD, GT * 128], BF16) for i in range(2)]
            mT_sc = [pb.new_dma_sc("mT%d" % i) for i in range(2)]
            xres = [sbuf(p6, "xres%d" % i, [128, D], F32) for i in range(2)]
            xr_sc = [pb.new_dma_sc("xr%d" % i) for i in range(2)]
            NB6 = 3
            h1 = [sbuf(p6, "h1_%d" % i, [128, D], F32) for i in range(NB6)]
            x1o = h1
            x1o_sc = [pb.new_dma_sc("x1o%d" % i) for i in range(NB6)]
            x1b = [sbuf(p6, "x1b%d" % i, [128, D], BF16) for i in range(NB6)]
            x1b_sc = [pb.new_dma_sc("x1b%d" % i) for i in range(NB6)]
            x1T = sbuf(p6, "x1T", [128, KD, 128], F32)
            st6b = sbuf(p6, "st6b", [128, max(1, D // 512), 6], F32)
            mv4 = sbuf(p6, "mv4", [128, 4], F32)
            G_, E_ = c["G"], c["E"]
            lga = sbuf(p6, "lga", [128, NT, NR], F32)
            gsh = sbuf(p6, "gsh", [128, NT, G_], F32)
            ohg = sbuf(p6, "ohg", [128, NT, G_], F32)
            elog = sbuf(p6, "elog", [128, NT, E_], F32)
            elog2 = sbuf(p6, "elog2", [128, NT, E_], F32)
            tmpE = sbuf(p6, "tmpE", [128, NT, E_], F32)
            oh1l = sbuf(p6, "oh1l", [128, NT, E_], F32)
            oh2l = sbuf(p6, "oh2l", [128, NT, E_], F32)
            sm = sbuf(p6, "sm", [128, 10, NT], F32)
            def p6_loads(tt):
                b = tt % 2
                gi = tt // GT
                gb = gi % 2
                if tt % GT == 0:
                    pb.dma(sp, mT_sc[gb], [("mergedT", j) for j in range(KD)], [("mTg", gb)],
                           lambda e: e.dma_start(out=mTg[gb][:], in_=mergedT[:, :, gi * GT * 128:(gi + 1) * GT * 128]
                                                 .rearrange("j p t -> p j t")))
                pb.dma(sp, xr_sc[b], [], [("xres", b)], lambda e: e.dma_start(out=xres[b][:], in_=x[tt * 128:(tt + 1) * 128, :]))

            def p6_A(tt):
                b = tt % 2
                hb3 = tt % NB6
                gi = tt // GT
                gb = gi % 2
                if tt + 1 < NT:
                    p6_loads(tt + 1)
                tl = (tt % GT) * 128
                for n in range(NQ):
                    bi, bk = ps_next()
                    pb.mm([("mTg", gb), ("wo", n)], [bk],
                          [lambda e, j=j: e.matmul(psum[:, bi, 0:qw], mTg[gb][:, j, tl:tl + 128], wo[:, j, n * qw:(n + 1) * qw],
                                                   start=(j == 0), stop=(j == KD - 1)) for j in range(KD)])
                    pb.op(dve, [bk, ("xres", b)], [("h1", hb3)],
                          lambda e: e.scalar_tensor_tensor(out=h1[hb3][:, n * qw:(n + 1) * qw], in0=xres[b][:, n * qw:(n + 1) * qw],
                                                           scalar=ALPHA, in1=psum[:, bi, 0:qw], op0=ALU.mult, op1=ALU.add))

            def p6_A2(tt):
                hb3 = tt % NB6
                ln_rows(h1[hb3][:], h1[hb3][:], ("h1", hb3), ("h1", hb3), st6b, mv4, gln[:], bln[:], ["gln", "bln"])
                pb.dma(sp, x1o_sc[hb3], [("h1", hb3)], [("X1F", tt)],
                       lambda e: e.dma_start(out=X1F[tt * 128:(tt + 1) * 128, :], in_=x1o[hb3][:]))
                pb.op(act, [("h1", hb3)], [("x1b", hb3)], lambda e: e.activation(out=x1b[hb3][:], in_=x1o[hb3][:], func=AF.Copy))
                pb.dma(sp, x1b_sc[hb3], [("x1b", hb3)], [("X1B", tt)],
                       lambda e: e.dma_start(out=X1B[tt * 128:(tt + 1) * 128, :], in_=x1b[hb3][:]))

            def p6_B(tt):
                hb3 = tt % NB6
                for k0 in range(0, KD, 4):
                    kn = min(4, KD - k0)
                    bi, bk = ps_next()
                    pb.mm([("h1", hb3), "idf"], [bk],
                          [lambda e, kk=kk: e.transpose(psum[:, bi, (kk - k0) * 128:(kk - k0 + 1) * 128],
                                                        x1o[hb3][:, kk * 128:(kk + 1) * 128], idf[:]) for kk in range(k0, k0 + kn)])
                    pb.op(act, [bk], [("x1T", k0)],
                          lambda e: e.activation(out=x1T[:, k0:k0 + kn, :],
                                                 in_=psum[:, bi, 0:kn * 128].rearrange("p (a b) -> p a b", a=kn), func=AF.Copy))
                bi, bk = ps_next()
                pb.mm([("x1T", k0) for k0 in range(0, KD, 4)] + ["wr"], [bk],
                      [lambda e, kc=kc: e.matmul(psum[:, bi, 0:NR], x1T[:, kc, :], wr[:, kc, :],
                                                 start=(kc == 0), stop=(kc == KD - 1)) for kc in range(KD)])
                pb.op(dve, [bk, "brt"], [("lga", tt)],
                      lambda e: e.tensor_tensor(out=lga[:, tt, :], in0=psum[:, bi, 0:NR], in1=brt[:], op=ALU.add))

            p6_loads(0)
            p6_A(0)
            p6_A2(0)
            if NT > 1:
                p6_A(1)
                p6_A2(1)
            for tt in range(NT):
                if tt + 2 < NT:
                    p6_A(tt + 2)
                p6_B(tt)
                if tt + 2 < NT:
                    p6_A2(tt + 2)

            lk = [("lga", tt) for tt in range(NT)]
            glog = lga[:, :, 0:G_]

            def bc(ap2, n):
                return ap2.unsqueeze(2).to_broadcast([128, NT, n])

            pb.op(dve, lk, ["gmax"], lambda e: e.reduce_max(out=sm[:, 0, :], in_=glog, axis=AX.X))
            pb.op(dve, lk + ["gmax"], ["gsh"], lambda e: e.tensor_tensor(out=gsh[:], in0=glog, in1=bc(sm[:, 0, :], G_), op=ALU.subtract))
            pb.op(act, ["gsh"], ["ohg"], lambda e: e.activation(out=ohg[:], in_=gsh[:], func=AF.Exp))
            pb.op(dve, ["ohg"], ["gsum"], lambda e: e.reduce_sum(out=sm[:, 1, :], in_=ohg[:], axis=AX.X))
            pb.op(dve, ["gsum"], ["gw"], lambda e: e.reciprocal(out=sm[:, 2, :], in_=sm[:, 1, :]))
            pb.op(dve, ["gsh", "ohg", "gsum"], ["ohg"],
                  lambda e: e.tensor_scalar(out=ohg[:], in0=gsh[:], scalar1=0.0, scalar2=None, op0=ALU.is_equal))
            for g in range(G_):
                src = lga[:, :, G_ + g * E_: G_ + (g + 1) * E_]
                dst = elog if g == 0 else tmpE
                pb.op(dve, lk + ["ohg", "tmpE", "elog"], ["tmpE" if g else "elog"],
                      lambda e: e.tensor_tensor(out=dst[:], in0=src, in1=bc(ohg[:, :, g], E_), op=ALU.mult))
                if g:
                    pb.op(dve, ["tmpE", "elog"], ["elog"], lambda e: e.tensor_tensor(out=elog[:], in0=elog[:], in1=tmpE[:], op=ALU.add))
            pb.op(dve, ["elog"], ["v1"], lambda e: e.reduce_max(out=sm[:, 3, :], in_=elog[:], axis=AX.X))
            pb.op(dve, ["elog", "v1"], ["oh1l"], lambda e: e.tensor_tensor(out=oh1l[:], in0=elog[:], in1=bc(sm[:, 3, :], E_), op=ALU.is_equal))
            pb.op(dve, ["elog", "oh1l"], ["elog2"],
                  lambda e: e.scalar_tensor_tensor(out=elog2[:], in0=oh1l[:], scalar=-1.0e30, in1=elog[:], op0=ALU.mult, op1=ALU.add))
            pb.op(dve, ["elog2"], ["v2"], lambda e: e.reduce_max(out=sm[:, 4, :], in_=elog2[:], axis=AX.X))
            pb.op(dve, ["elog2", "v2"], ["oh2l"], lambda e: e.tensor_tensor(out=oh2l[:], in0=elog2[:], in1=bc(sm[:, 4, :], E_), op=ALU.is_equal))
            pb.op(dve, ["v1", "v2"], ["e21"], lambda e: e.tensor_tensor(out=sm[:, 5, :], in0=sm[:, 4, :], in1=sm[:, 3, :], op=ALU.subtract))
            pb.op(act, ["e21"], ["e21"], lambda e: e.activation(out=sm[:, 5, :], in_=sm[:, 5, :], func=AF.Exp))
            pb.op(dve, ["e21"], ["den"], lambda e: e.tensor_scalar(out=sm[:, 6, :], in0=sm[:, 5, :], scalar1=1.0, scalar2=None, op0=ALU.add))
            pb.op(dve, ["den"], ["w1"], lambda e: e.reciprocal(out=sm[:, 7, :], in_=sm[:, 6, :]))
            pb.op(dve, ["w1", "e21"], ["w2"], lambda e: e.tensor_tensor(out=sm[:, 8, :], in0=sm[:, 7, :], in1=sm[:, 5, :], op=ALU.mult))
            for k in range(2):
                pb.op(dve, ["w1", "w2", "gw"], [("w12", k)],
                      lambda e: e.tensor_tensor(out=w12[:, :, k], in0=sm[:, 7 + k, :], in1=sm[:, 2, :], op=ALU.mult))
            for g in range(G_):
                pb.op(dve, ["oh1l", "ohg"], [("OH1g", g)],
                      lambda e: e.tensor_tensor(out=OH1[:, :, g * E_:(g + 1) * E_], in0=oh1l[:], in1=bc(ohg[:, :, g], E_), op=ALU.mult))
                pb.op(dve, ["oh2l", "ohg"], [("OH2g", g)],
                      lambda e: e.tensor_tensor(out=OH2[:, :, g * E_:(g + 1) * E_], in0=oh2l[:], in1=bc(ohg[:, :, g], E_), op=ALU.mult))
            pb.op(dve, [("OH1g", g) for g in range(G_)] + [("OH2g", g) for g in range(G_)], ["OHs"],
                  lambda e: e.tensor_tensor(out=OHs[:], in0=OH1[:], in1=OH2[:], op=ALU.add))
            pb.barrier()

        if debug == "x1":
            sp.wait([(sc_, sc_.n) for sc_ in x1o_sc])
            return nc, c

        scat_sc = [pb.new_dma_sc("scat%d" % i) for i in range(2)]
        with ExitStack() as p6b:
            cntB = sbuf(p6b, "cntB", [128, NE], F32)
            ntl = sbuf(p6b, "ntl", [128, NE], F32)
            scb = [sbuf(p6b, "scb%d" % i, [128, NE], F32) for i in range(2)]
            tef = sbuf(p6b, "tef", [128, NTILE], F32)
            iot = sbuf(p6b, "iot", [128, NTILE], F32)
            base = sbuf(p6b, "base", [128, NE], F32)
            prod = sbuf(p6b, "prod", [128, NE], F32)
            xb = [sbuf(p6b, "xb%d" % i, [128, D], BF16) for i in range(2)]
            xb_sc = [pb.new_dma_sc("xb%d" % i) for i in range(2)]
            io_sc = pb.new_dma_sc("iot")
            pb.dma(sp, io_sc, [], ["iot"], lambda e: e.dma_start(out=iot[:], in_=iota_t))
            oh_keys = [("OHs", tt) for tt in range(NT)]
            bi, bk = ps_next()
            pb.mm(oh_keys + ["ones_f"], [bk],
                  [lambda e, tt=tt: e.matmul(psum[:, bi, 0:NE], ones_f[:], OHs[:, tt, :], start=(tt == 0), stop=(tt == NT - 1))
                   for tt in range(NT)])
            pb.op(dve, [bk], ["cntB"], lambda e: e.tensor_copy(out=cntB[:], in_=psum[:, bi, 0:NE]))
            MAXT = S // TS
            pb.op(dve, ["cntB"], ["ntl"], lambda e: e.tensor_scalar(out=ntl[:], in0=cntB[:], scalar1=0.0, scalar2=None, op0=ALU.is_gt))
            for m in range(1, MAXT):
                pb.op(dve, ["cntB", "ntl"], ["ntl"],
                      lambda e: e.scalar_tensor_tensor(out=ntl[:], in0=cntB[:], scalar=float(m * TS), in1=ntl[:],
                                                       op0=ALU.is_gt, op1=ALU.add))
            cur, curk = ntl, "ntl"
            sh = 1
            ib = 0
            while sh < NE:
                nxt, nxtk = scb[ib], ("scb", ib)
                pb.op(dve, [curk], [nxtk], lambda e: e.tensor_copy(out=nxt[:, 0:sh], in_=cur[:, 0:sh]))
                pb.op(dve, [curk, nxtk], [nxtk],
                      lambda e: e.tensor_tensor(out=nxt[:, sh:NE], in0=cur[:, sh:NE], in1=cur[:, 0:NE - sh], op=ALU.add))
                cur, curk = nxt, nxtk
                ib = 1 - ib
                sh *= 2
            pb.op(dve, [curk, "ntl"], ["toff"], lambda e: e.tensor_tensor(out=toff[:], in0=cur[:], in1=ntl[:], op=ALU.subtract))
            pb.op(dve, ["toff"], ["soff"], lambda e: e.tensor_scalar(out=soff[:], in0=toff[:], scalar1=float(TS), scalar2=None, op0=ALU.mult))
            pb.op(dve, [], ["tef"], lambda e: e.memset(tef[:], -1.0))
            for ex in range(NE):
                pb.op(dve, ["toff", "iot", "tef"], ["tef"],
                      lambda e: e.scalar_tensor_tensor(out=tef[:], in0=iot[:], scalar=toff[:, ex:ex + 1], in1=tef[:],
                                                       op0=ALU.is_ge, op1=ALU.add))
            padf = sbuf(p6b, "padf", [128, NTILE], F32)
            pb.op(dve, [curk, "iot"], ["padf"],
                  lambda e: e.tensor_scalar(out=padf[:], in0=iot[:], scalar1=cur[:, NE - 1:NE], scalar2=float(NE),
                                            op0=ALU.is_ge, op1=ALU.mult))
            pb.op(dve, ["padf", "tef"], ["tef"], lambda e: e.tensor_tensor(out=tef[:], in0=tef[:], in1=padf[:], op=ALU.add))
            pb.op(dve, ["tef", "iop"], ["tef"],
                  lambda e: e.tensor_scalar(out=tef[:], in0=tef[:], scalar1=128.0, scalar2=iop[:, 0:1], op0=ALU.mult, op1=ALU.add))
            pb.op(dve, ["tef"], ["tei"], lambda e: e.tensor_copy(out=tei[:], in_=tef[:]))
            for tt in range(NT):
                b = tt % 2
                bi, bk = ps_next()
                fns = [lambda e: e.matmul(psum[:, bi, 0:NE], trif[:], OHs[:, tt, :], start=True, stop=(tt == 0))]
                for t2 in range(tt):
                    fns.append(lambda e, t2=t2: e.matmul(psum[:, bi, 0:NE], ones_f[:], OHs[:, t2, :], start=False, stop=(t2 == tt - 1)))
                pb.mm(oh_keys + ["trif", "ones_f"], [bk], fns)
                pb.op(dve, [bk, "soff"], ["base"], lambda e: e.tensor_tensor(out=base[:], in0=psum[:, bi, 0:NE], in1=soff[:], op=ALU.add))
                for k, OHk, ohn in ((0, OH1, "OH1"), (1, OH2, "OH2")):
                    pb.op(dve, ["base", (ohn, tt), "prod"], ["prod"],
                          lambda e: e.tensor_tensor(out=prod[:], in0=base[:], in1=OHk[:, tt, :], op=ALU.mult))
                    pb.op(dve, ["prod"], [("slf", tt, k)], lambda e: e.reduce_sum(out=slf[:, tt, k:k + 1], in_=prod[:], axis=AX.X))
                pb.op(dve, [("slf", tt, 0), ("slf", tt, 1)], [("sli", tt)], lambda e: e.tensor_copy(out=sli[:, tt, :], in_=slf[:, tt, :]))
                pb.dma(sp, xb_sc[b], [], [("xb", b)], lambda e: e.dma_start(out=xb[b][:], in_=X1B[tt * 128:(tt + 1) * 128, :]))
                pool.wait([zf_box[0]])
                pb.dma_group(pool, scat_sc[b], [("xb", b), ("sli", tt)], [],
                             [lambda e, k=k: e.indirect_dma_start(
                                 out=Xs, out_offset=bass.IndirectOffsetOnAxis(ap=sli[:, tt, k:k + 1], axis=0),
                                 in_=xb[b][:], in_offset=None) for k in range(2)])
            pb.barrier()

        if debug == "route":
            dsl = nc.dram_tensor("dsl", [128, NT, 2], I32, kind="ExternalOutput").ap()
            dw = nc.dram_tensor("dw", [128, NT, 2], F32, kind="ExternalOutput").ap()
            dte = nc.dram_tensor("dte", [128, NTILE], I32, kind="ExternalOutput").ap()
            dsc = pb.new_dma_sc("dbg")
            pb.dma(sp, dsc, [], [], lambda e: e.dma_start(out=dsl, in_=sli[:]))
            pb.dma(sp, dsc, [], [], lambda e: e.dma_start(out=dw, in_=w12[:]))
            pb.dma(sp, dsc, [], [], lambda e: e.dma_start(out=dte, in_=tei[:]))
            sp.wait([(dsc, dsc.n)])
            return nc, c

        ys_scs = []
        with ExitStack() as p7:
            Wg = [sbuf(p7, "Wg%d" % i, [128, KD, F], BF16) for i in range(2)]
            Wu = [sbuf(p7, "Wu%d" % i, [128, KD, F], BF16) for i in range(2)]
            Wd = [sbuf(p7, "Wd%d" % i, [128, FC, D], BF16) for i in range(2)]
            w_sc = [pb.new_dma_sc("W%d" % i) for i in range(2)]
            xst = [sbuf(p7, "xst%d" % i, [128, SUB, D], BF16) for i in range(2)]
            xst_sc = [pb.new_dma_sc("xst%d" % i) for i in range(2)]
            XsT = [sbuf(p7, "XsT%d" % i, [128, KD, TS], BF16) for i in range(2)]
            hT = [sbuf(p7, "hT%d" % i, [128, FC, TS], BF16) for i in range(2)]
            sa = [sbuf(p7, "sa%d" % i, [128, TS], F32) for i in range(2)]
            ysb = [sbuf(p7, "ysb%d" % i, [128, D], F32) for i in range(2)]
            ys_sc = [pb.new_dma_sc("ys%d" % i) for i in range(2)]
            ys_scs = ys_sc
            sa_ring = Ring(2)
            ys_ring = Ring(2)
            bnd_reg = nc.gpsimd.alloc_register("wbound")
            nc.gpsimd.reg_mov(bnd_reg, NE * 128 - 1)
            NQ = max(1, D // 512)
            qw = D // NQ
            ev_alt = [0]
            def p7_xload(j):
                b = j % 2
                pb.dma(sp, xst_sc[b], [], [("xst", b)],
                       lambda e: e.dma_start(out=xst[b][:], in_=Xs[j * TS:(j + 1) * TS, :].rearrange("(s p) d -> p s d", p=128)))

            def p7_W(j):
                b = j % 2
                pb.dma_group(pool, w_sc[b], ["tei"], [("W", b)],
                             [lambda e, dst=dst, srcw=srcw: e.indirect_dma_start(
                                 out=dst[:].rearrange("p a b -> p (a b)"), out_offset=None, in_=srcw,
                                 in_offset=bass.IndirectOffsetOnAxis(ap=tei[:, j:j + 1], axis=0),
                                 bounds_check=bnd_reg, oob_is_err=False)
                              for (dst, srcw) in ((Wg[b], w_gate), (Wu[b], w_up), (Wd[b], w_down))])
            def p7_T(j):
                b = j % 2
                if j + 1 < NTILE:
                    p7_xload(j + 1)
                for sub in range(SUB):
                    for k0 in range(0, KD, 8):
                        kn = min(8, KD - k0)
                        bi, bk = ps_next()
                        pbf = psum[:, bi, :].bitcast(BF16)
                        pb.mm([("xst", b), "idb"], [bk],
                              [lambda e, kk=kk: e.transpose(pbf[:, (kk - k0) * 128:(kk - k0 + 1) * 128],
                                                            xst[b][:, sub, kk * 128:(kk + 1) * 128], idb[:]) for kk in range(k0, k0 + kn)])
                        ev_alt[0] ^= 1
                        src_ap = pbf[:, 0:kn * 128].rearrange("p (a b) -> p a b", a=kn)
                        dst_ap = XsT[b][:, k0:k0 + kn, sub * 128:(sub + 1) * 128]
                        if ev_alt[0]:
                            pb.op(act, [bk], [("XsT", b, sub, k0)], lambda e: e.activation(out=dst_ap, in_=src_ap, func=AF.Copy))
                        else:
                            pb.op(dve, [bk], [("XsT", b, sub, k0)], lambda e: e.tensor_copy(out=dst_ap, in_=src_ap))
            def p7_GU(j):
                b = j % 2
                xk = [("XsT", b, sub, k0) for sub in range(SUB) for k0 in range(0, KD, 8)]
                for fc in range(FC):
                    ba, bka = ps_next()
                    pb.mm(xk + [("W", b)], [bka],
                          [lambda e, kc=kc: e.matmul(psum[:, ba, 0:TS], Wg[b][:, kc, fc * 128:(fc + 1) * 128], XsT[b][:, kc, :],
                                                     start=(kc == 0), stop=(kc == KD - 1)) for kc in range(KD)])
                    si = sa_ring.next()
                    pb.op(act, [bka], [("sa", si)], lambda e: e.activation(out=sa[si][:], in_=psum[:, ba, 0:TS], func=AF.Silu))
                    bu, bku = ps_next()
                    pb.mm(xk + [("W", b)], [bku],
                          [lambda e, kc=kc: e.matmul(psum[:, bu, 0:TS], Wu[b][:, kc, fc * 128:(fc + 1) * 128], XsT[b][:, kc, :],
                                                     start=(kc == 0), stop=(kc == KD - 1)) for kc in range(KD)])
                    pb.op(dve, [bku, ("sa", si)], [("hT", b, fc)],
                          lambda e: e.tensor_tensor(out=hT[b][:, fc, :], in0=psum[:, bu, 0:TS], in1=sa[si][:], op=ALU.mult))
            def p7_D(j):
                b = j % 2
                hk = [("hT", b, fc) for fc in range(FC)]
                for sub in range(SUB):
                    yi = ys_ring.next()
                    for n in range(NQ):
                        bo, bko = ps_next()
                        pb.mm(hk + [("W", b)], [bko],
                              [lambda e, fc=fc: e.matmul(psum[:, bo, 0:qw], hT[b][:, fc, sub * 128:(sub + 1) * 128],
                                                         Wd[b][:, fc, n * qw:(n + 1) * qw], start=(fc == 0), stop=(fc == FC - 1))
                               for fc in range(FC)])
                        if n % 2 == 0:
                            pb.op(act, [bko], [("ysb", yi)],
                                  lambda e: e.activation(out=ysb[yi][:, n * qw:(n + 1) * qw], in_=psum[:, bo, 0:qw], func=AF.Copy))
                        else:
                            pb.op(dve, [bko], [("ysb", yi)],
                                  lambda e: e.tensor_copy(out=ysb[yi][:, n * qw:(n + 1) * qw], in_=psum[:, bo, 0:qw]))
                    r0 = j * TS + sub * 128
                    pb.dma(sp, ys_sc[yi], [("ysb", yi)], [], lambda e: e.dma_start(out=Ys[r0:r0 + 128, :], in_=ysb[yi][:]))

            p7_W(0)
            p7_xload(0)
            p7_T(0)
            for j in range(NTILE):
                if j + 1 < NTILE:
                    p7_W(j + 1)
                p7_GU(j)
                if j + 1 < NTILE:
                    p7_T(j + 1)
                p7_D(j)
            pb.barrier()

        with ExitStack() as p8:
            y1 = [sbuf(p8, "y1_%d" % i, [128, D], F32) for i in range(2)]
            y2 = [sbuf(p8, "y2_%d" % i, [128, D], F32) for i in range(2)]
            xf = [sbuf(p8, "xf%d" % i, [128, D], F32) for i in range(2)]
            ho = [sbuf(p8, "ho%d" % i, [128, D], F32) for i in range(2)]
            y1_sc = [pb.new_dma_sc("y1_%d" % i) for i in range(2)]
            y2_sc = [pb.new_dma_sc("y2_%d" % i) for i in range(2)]
            xf_sc = [pb.new_dma_sc("xf%d" % i) for i in range(2)]
            out_sc = [pb.new_dma_sc("out%d" % i) for i in range(2)]
            st6c = sbuf(p8, "st6c", [128, max(1, D // 512), 6], F32)
            mv4c = sbuf(p8, "mv4c", [128, 4], F32)
            c3_sc = pb.new_dma_sc("c3")
            pb.dma(sp, c3_sc, [], ["gln"], lambda e: e.dma_start(out=gln[:], in_=ln2_g))
            pb.dma(sp, c3_sc, [], ["bln"], lambda e: e.dma_start(out=bln[:], in_=ln2_b))
            c3_tok = (c3_sc, c3_sc.n)
            pb.deps["gln"].w = c3_tok
            pb.deps["bln"].w = c3_tok
            def p8_loads(tt):
                b = tt % 2
                pb.dma(pool, y1_sc[b], [], [("y1", b)],
                       lambda e: e.indirect_dma_start(out=y1[b][:], out_offset=None, in_=Ys,
                                                      in_offset=bass.IndirectOffsetOnAxis(ap=sli[:, tt, 0:1], axis=0)))
                pb.dma(pool, y2_sc[b], [], [("y2", b)],
                       lambda e: e.indirect_dma_start(out=y2[b][:], out_offset=None, in_=Ys,
                                                      in_offset=bass.IndirectOffsetOnAxis(ap=sli[:, tt, 1:2], axis=0)))
                pb.dma(sp, xf_sc[b], [], [("xf", b)], lambda e: e.dma_start(out=xf[b][:], in_=X1F[tt * 128:(tt + 1) * 128, :]))

            p8_loads(0)
            for tt in range(NT):
                b = tt % 2
                if tt + 1 < NT:
                    p8_loads(tt + 1)
                pb.op(act, [("xf", b)], [("xf", b)], lambda e: e.activation(out=xf[b][:], in_=xf[b][:], func=AF.Copy, scale=ALPHA))
                pb.op(dve, [("xf", b), ("y1", b)], [("xf", b)],
                      lambda e: e.scalar_tensor_tensor(out=xf[b][:], in0=y1[b][:], scalar=w12[:, tt, 0:1], in1=xf[b][:],
                                                       op0=ALU.mult, op1=ALU.add))
                pb.op(dve, [("xf", b), ("y2", b)], [("xf", b)],
                      lambda e: e.scalar_tensor_tensor(out=xf[b][:], in0=y2[b][:], scalar=w12[:, tt, 1:2], in1=xf[b][:],
                                                       op0=ALU.mult, op1=ALU.add))
                ln_rows(xf[b][:], ho[b][:], ("xf", b), ("ho", b), st6c, mv4c, gln[:], bln[:], ["gln", "bln"], mul_eng=dve)
                pb.dma(sp, out_sc[b], [("ho", b)], [], lambda e: e.dma_start(out=out[tt * 128:(tt + 1) * 128, :], in_=ho[b][:]))
            sp.wait([(sc_, sc_.n) for sc_ in out_sc])
            pb.barrier()
    return nc, c


def const_tables(c):
    NG, HPG = c["NG"], c["HPG"]
    n = NG * HPG
    slopes = (2.0 ** (-ALIBI_MAX * np.arange(1, n + 1, dtype=np.float32) / n)).reshape(NG, HPG)
    k = np.arange(128)[:, None].astype(np.float64)
    q = np.arange(128)[None, :].astype(np.float64)
    ebt = np.zeros((n, 128, 2, 128), np.float32)
    for g, (window, dil) in enumerate(c["PATTERNS"]):
        assert window // dil == 128
        for hh in range(HPG):
            s = float(slopes[g, hh]) * dil
            cur = np.where(k <= q, np.exp(-s * (q - k)), 0.0)
            prev = np.where(k >= q, np.exp(-s * (q + 128 - k)), 0.0)
            ebt[g * HPG + hh, :, 0, :] = cur
            ebt[g * HPG + hh, :, 1, :] = prev
    tri = (np.arange(128)[:, None] < np.arange(128)[None, :]).astype(np.float32)
    return dict(
        ident_f=np.eye(128, dtype=np.float32),
        ident_b=np.eye(128, dtype=np.float32).astype(ml_dtypes.bfloat16),
        ebt=ebt, tri=tri,
        iota_e=np.broadcast_to(np.arange(c["NE"], dtype=np.float32), (128, c["NE"])).copy(),
        iota_t=np.broadcast_to(np.arange(c["NTILE"], dtype=np.float32), (128, c["NTILE"])).copy(),
        iota_p=np.arange(128, dtype=np.float32).reshape(128, 1).copy(),
    )


def ex_layout(w):
    ne, k, n = w.shape
    return np.ascontiguousarray(w.reshape(ne, k // 128, 128, n).transpose(0, 2, 1, 3)).reshape(ne * 128, (k // 128) * n)


def prep_shared(c, inp):
    f = lambda a: np.ascontiguousarray(np.asarray(a, dtype=np.float32))
    rep = lambda v: np.ascontiguousarray(np.broadcast_to(f(v).reshape(1, -1), (128, f(v).size)))
    L = 0
    G, E, D = c["G"], c["E"], c["D"]
    w_r = np.concatenate([f(inp["w_group"])[L], f(inp["w_router"])[L].transpose(1, 0, 2).reshape(D, G * E)], axis=1)
    b_r = np.concatenate([f(inp["b_group"])[L].reshape(-1), f(inp["b_router"])[L].reshape(-1)])
    sh = dict(
        lnm_g=rep(inp["ln_mem_g"]), lnm_b=rep(inp["ln_mem_b"]),
        w_in=f(inp["w_in"])[L],
        b_col=np.ascontiguousarray(f(inp["b_in"])[L].reshape(-1, 128).T),
        wconv_col=np.ascontiguousarray(f(inp["w_conv"])[L].reshape(3, -1, 128).transpose(2, 0, 1)),
        w_conv_out=f(inp["w_conv_out"])[L], w_dil_out=f(inp["w_dil_out"])[L], w_mem_kv=f(inp["w_mem_kv"])[L],
        w_mem_out=f(inp["w_mem_out"])[L], w_o=f(inp["w_o"])[L],
        ln1_g=rep(f(inp["ln1_g"])[L]), ln1_b=rep(f(inp["ln1_b"])[L]),
        w_r=np.ascontiguousarray(w_r), b_r=rep(b_r),
        w_gate=ex_layout(f(inp["w_gate"])[L].reshape(G * E, D, c["F"])),
        w_up=ex_layout(f(inp["w_up"])[L].reshape(G * E, D, c["F"])),
        w_down=ex_layout(f(inp["w_down"])[L].reshape(G * E, c["F"], D)),
        ln2_g=rep(f(inp["ln2_g"])[L]), ln2_b=rep(f(inp["ln2_b"])[L]),
    )
    sh.update(const_tables(c))
    return sh


def make_in_maps(c, inp):
    sh = prep_shared(c, inp)
    xs = np.asarray(inp["x"], dtype=np.float32)
    ms = np.asarray(inp["mem"], dtype=np.float32)
    maps = []
    for b in range(xs.shape[0]):
        m = dict(sh)
        m["x"] = np.ascontiguousarray(xs[b])
        m["mem"] = np.ascontiguousarray(ms[b])
        maps.append(m)
    return maps


def kernel(**inputs):
    c = derive(FULL)
    nc, _ = build_program(FULL)
    maps = make_in_maps(c, inputs)
    res = run_bass_kernel_spmd(nc, maps, core_ids=list(range(len(maps))))
    return np.stack([np.asarray(r["out"], dtype=np.float32) for r in res.results], axis=0)
```

```python
import numpy as np
import ml_dtypes
from contextlib import ExitStack
import concourse.bass as bass
import concourse.mybir as mybir
from concourse.bass_utils import run_bass_kernel_spmd

F32 = mybir.dt.float32
BF16 = mybir.dt.bfloat16
I32 = mybir.dt.int32
U32 = mybir.dt.uint32
AF = mybir.ActivationFunctionType
ALU = mybir.AluOpType
AX = mybir.AxisListType

LN_EPS = 1e-5
ALIBI_MAX = 8.0

FULL = dict(D=2048, S=2048, CONV=1024, PATTERNS=((128, 1), (512, 4), (2048, 16)), HPG=4,
            ML=256, MEM_HEADS=4, MEM_HD=256, G=4, E=8, F=512, TS=256, DEPTH=1)


def derive(cfg):
    c = dict(cfg)
    c["NG"] = len(c["PATTERNS"])
    c["DIL"] = c["NG"] * c["HPG"] * 128
    c["DIL_OUT"] = c["HPG"] * 128
    c["MEM_DIM"] = c["MEM_HEADS"] * c["MEM_HD"]
    c["IN_DIM"] = 3 * c["CONV"] + 3 * c["DIL"] + c["MEM_DIM"] + 3 * c["D"]
    c["KD"] = c["D"] // 128
    c["NT"] = c["S"] // 128
    c["TGN"] = c["S"] // 512
    c["cB"] = 0
    c["cC"] = c["CONV"]
    c["cH"] = 2 * c["CONV"]
    c["cQ"] = 3 * c["CONV"]
    c["cK"] = c["cQ"] + c["DIL"]
    c["cV"] = c["cK"] + c["DIL"]
    c["cMQ"] = c["cV"] + c["DIL"]
    c["cG"] = c["cMQ"] + c["MEM_DIM"]
    c["NE"] = c["G"] * c["E"]
    c["NR"] = c["G"] + c["NE"]
    c["NTILE"] = (2 * c["S"]) // c["TS"] + c["NE"]
    c["NSLOT"] = c["NTILE"] * c["TS"]
    c["ALPHA"] = (2 * c["DEPTH"]) ** 0.25
    return c


class SemC:
    def __init__(self, h):
        self.h = h
        self.n = 0


class EngH:
    def __init__(self, pb, eng, name):
        self.pb = pb
        self.e = eng
        self.name = name
        self.sc = SemC(pb.newsem("s_" + name))
        self.seen = {}

    def wait(self, toks):
        best = {}
        for t in toks:
            if t is None:
                continue
            sc, v = t
            if self.seen.get(id(sc), 0) >= v:
                continue
            if best.get(id(sc), (None, 0))[1] < v:
                best[id(sc)] = (sc, v)
        for sc, v in best.values():
            self.e.wait_ge(sc.h, v)
            self.seen[id(sc)] = v

    def mark(self, ins):
        ins.then_inc(self.sc.h, 1)
        self.sc.n += 1
        return (self.sc, self.sc.n)


class Dep:
    __slots__ = ("w", "r")

    def __init__(self):
        self.w = None
        self.r = {}


class PB:
    def __init__(self, nc):
        self.nc = nc
        self.es = ExitStack()
        self.deps = {}
        self.nsem = 0
        self.pe = EngH(self, nc.tensor, "pe")
        self.act = EngH(self, nc.scalar, "act")
        self.dve = EngH(self, nc.vector, "dve")
        self.pool = EngH(self, nc.gpsimd, "pool")
        self.sp = EngH(self, nc.sync, "sp")
        self.engs = [self.pe, self.act, self.dve, self.pool, self.sp]
        self.dma_scs = []

    def newsem(self, name):
        self.nsem += 1
        return self.es.enter_context(self.nc.semaphore(name + "_%d" % self.nsem))

    def new_dma_sc(self, name):
        sc = SemC(self.newsem("d_" + name))
        self.dma_scs.append(sc)
        return sc

    def _collect(self, reads, writes):
        toks = []
        for k in reads:
            d = self.deps.get(k)
            if d is not None and d.w is not None:
                toks.append(d.w)
        for k in writes:
            d = self.deps.get(k)
            if d is not None:
                if d.w is not None:
                    toks.append(d.w)
                toks.extend(d.r.values())
        return toks

    def _commit(self, tok, reads, writes):
        for k in reads:
            d = self.deps.setdefault(k, Dep())
            cur = d.r.get(id(tok[0]))
            if cur is None or cur[1] < tok[1]:
                d.r[id(tok[0])] = tok
        for k in writes:
            d = self.deps.setdefault(k, Dep())
            d.w = tok
            d.r = {}

    def op(self, eng, reads, writes, fn, extra=()):
        eng.wait(self._collect(reads, writes) + list(extra))
        tok = eng.mark(fn(eng.e))
        self._commit(tok, reads, writes)
        return tok

    def mm(self, reads, writes, fns):
        eng = self.pe
        eng.wait(self._collect(reads, writes))
        ins = None
        for fn in fns:
            ins = fn(eng.e)
        tok = eng.mark(ins)
        self._commit(tok, reads, writes)
        return tok

    def dma(self, eng, sc, reads, writes, fn):
        eng.wait(self._collect(reads, writes))
        ins = fn(eng.e)
        ins.then_inc(sc.h, 16)
        sc.n += 16
        tok = (sc, sc.n)
        self._commit(tok, reads, writes)
        return tok

    def dma_group(self, eng, sc, reads, writes, fns):
        eng.wait(self._collect(reads, writes))
        for fn in fns:
            ins = fn(eng.e)
            ins.then_inc(sc.h, 16)
            sc.n += 16
        tok = (sc, sc.n)
        self._commit(tok, reads, writes)
        return tok

    def barrier(self, keep=()):
        toks = [(e.sc, e.sc.n) for e in self.engs if e.sc.n > 0]
        toks += [(sc, sc.n) for sc in self.dma_scs if sc.n > 0]
        for e in self.engs:
            e.wait(toks)
        self.deps = {k: v for k, v in self.deps.items() if k in keep}


class Ring:
    def __init__(self, n):
        self.n = n
        self.i = -1

    def next(self):
        self.i += 1
        return self.i % self.n


def build_program(cfg, debug=None):
    c = derive(cfg)
    D, S, KD, NT, TGN = c["D"], c["S"], c["KD"], c["NT"], c["TGN"]
    CONV, DIL, DIL_OUT, MEM_DIM, ML = c["CONV"], c["DIL"], c["DIL_OUT"], c["MEM_DIM"], c["ML"]
    HPG, NG, IN_DIM = c["HPG"], c["NG"], c["IN_DIM"]
    NE, NR, F, TS, NTILE, NSLOT = c["NE"], c["NR"], c["F"], c["TS"], c["NTILE"], c["NSLOT"]
    ALPHA = c["ALPHA"]
    NH = NG * HPG
    KC_CONV, KC_DO, KC_MEM = CONV // 128, DIL_OUT // 128, MEM_DIM // 128
    MCH = ML // 128
    HC = c["MEM_HD"] // 128
    FC = F // 128
    SUB = TS // 128

    nc = bass.Bass("TRN2", target_bir_lowering=False)

    def din(name, shape, dt=F32):
        return nc.dram_tensor(name, list(shape), dt, kind="ExternalInput").ap()

    x = din("x", [S, D])
    mem = din("mem", [ML, D])
    lnm_g = din("lnm_g", [128, D])
    lnm_b = din("lnm_b", [128, D])
    w_in = din("w_in", [D, IN_DIM])
    b_col = din("b_col", [128, IN_DIM // 128])
    wconv_col = din("wconv_col", [128, 3, CONV // 128])
    w_conv_out = din("w_conv_out", [CONV, D])
    w_dil_out = din("w_dil_out", [DIL_OUT, D])
    w_mem_kv = din("w_mem_kv", [D, 2 * MEM_DIM])
    w_mem_out = din("w_mem_out", [MEM_DIM, D])
    w_o = din("w_o", [D, D])
    ln1_g = din("ln1_g", [128, D])
    ln1_b = din("ln1_b", [128, D])
    w_r = din("w_r", [D, NR])
    b_r = din("b_r", [128, NR])
    w_gate = din("w_gate", [NE * 128, KD * F])
    w_up = din("w_up", [NE * 128, KD * F])
    w_down = din("w_down", [NE * 128, (F // 128) * D])
    iota_p = din("iota_p", [128, 1])
    ln2_g = din("ln2_g", [128, D])
    ln2_b = din("ln2_b", [128, D])
    ident_f = din("ident_f", [128, 128])
    ident_b = din("ident_b", [128, 128], BF16)
    ebt = din("ebt", [NH, 128, 2, 128])
    tri = din("tri", [128, 128])
    iota_e = din("iota_e", [128, NE])
    iota_t = din("iota_t", [128, NTILE])

    out = nc.dram_tensor("out", [S, D], F32, kind="ExternalOutput").ap()
    mergedT = nc.dram_tensor("mergedT", [KD, 128, S], BF16,
                             kind="ExternalOutput" if debug == "merged" else "Internal").ap()

    X1F = nc.dram_tensor("X1F", [S, D], F32, kind="ExternalOutput" if debug == "x1" else "Internal").ap()
    X1B = nc.dram_tensor("X1B", [S, D], BF16, kind="Internal").ap()
    Xs = nc.dram_tensor("Xs", [NSLOT, D], BF16, kind="ExternalOutput" if debug == "route" else "Internal").ap()
    Ys = nc.dram_tensor("Ys", [NSLOT, D], F32, kind="Internal").ap()

    pb = PB(nc)
    pe, act, dve, pool, sp = pb.pe, pb.act, pb.dve, pb.pool, pb.sp

    with pb.es:
        top = pb.es

        def sbuf(stack, name, shape, dt):
            return stack.enter_context(nc.sbuf_tensor(name, list(shape), dt))

        psum = top.enter_context(nc.psum_tensor("psum", [128, 8, 512], F32))
        ps_ring = Ring(8)

        def ps_next():
            i = ps_ring.next()
            return i, ("ps", i)

        cst_sc = pb.new_dma_sc("cst")
        idf = sbuf(top, "idf", [128, 128], F32)
        idb = sbuf(top, "idb", [128, 128], BF16)
        ones_b = sbuf(top, "ones_b", [128, 128], BF16)
        bcol = sbuf(top, "bcol", [128, IN_DIM // 128], F32)
        wcc = sbuf(top, "wcc", [128, 3, CONV // 128], F32)
        for (dst, src, key) in ((idf, ident_f, "idf"), (idb, ident_b, "idb"), (bcol, b_col, "bcol"),
                                (wcc, wconv_col, "wcc")):
            pb.dma(sp, cst_sc, [], [key], lambda e, dst=dst, src=src: e.dma_start(out=dst[:], in_=src))
        pb.op(dve, [], ["ones_b"], lambda e: e.memset(ones_b[:], 1.0))
        zt = sbuf(top, "zt", [128, D], BF16)
        pb.op(dve, [], ["zt"], lambda e: e.memset(zt[:], 0.0))
        zf_sc = SemC(pb.newsem("d_zf"))
        NZ = NSLOT // 128
        ZG = 8
        zf_box = []

        def emit_zero_fill():
            zf_box.append(pb.dma_group(sp, zf_sc, ["zt"], [],
                          [lambda e, z0=z0: e.dma_start(
                              out=Xs[z0 * 128:min(NZ, z0 + ZG) * 128, :].rearrange("(n p) d -> p n d", p=128),
                              in_=zt[:].unsqueeze(1).to_broadcast([128, min(NZ, z0 + ZG) - z0, D]))
                           for z0 in range(0, NZ, ZG)]))

        WR = 6
        wbuf = []
        wsc = [pb.new_dma_sc("w%d" % i) for i in range(WR)]
        wring = Ring(WR)

        def wload(src, kc_n):
            s = wring.next()
            pb.dma(pool, wsc[s], [], [("wb", s)],
                   lambda e: e.dma_start(out=wbuf[s][:, 0:kc_n, :],
                                         in_=src.rearrange("(kc p) n -> p kc n", p=128)))
            return s

        def proj_fm(src, kc_n, rhs_fn, rhs_keys, n_groups, consume, n_cols=512, out_fn=None):
            s = wload(src, kc_n)
            for g in range(n_groups):
                bi, bk = ps_next()
                o_ap = psum[:, bi, 0:n_cols] if out_fn is None else out_fn(psum[:, bi, 0:n_cols])
                pb.mm([("wb", s)] + list(rhs_keys), [bk],
                      [lambda e, kc=kc, g=g, bi=bi: e.matmul(
                          o_ap, wbuf[s][:, kc, :], rhs_fn(kc, g),
                          start=(kc == 0), stop=(kc == kc_n - 1)) for kc in range(kc_n)])
                consume(g, bi, bk)

        with ExitStack() as s1:
            wbuf.extend(sbuf(s1, "wb%d" % i, [128, KD, 128], BF16) for i in range(WR))
            xT = sbuf(s1, "xT", [128, KD, S], BF16)
            mkT = sbuf(s1, "mkT", [128, KC_MEM, ML], BF16)
            mvt = sbuf(s1, "mvt", [128, MCH, MEM_DIM], BF16)

            with ExitStack() as p0:
                memx = sbuf(p0, "memx", [128, MCH, D], F32)
                memn = sbuf(p0, "memn", [128, MCH, D], BF16)
                memnT = sbuf(p0, "memnT", [128, KD, ML], BF16)
                lng = sbuf(p0, "lng", [128, D], F32)
                lnb = sbuf(p0, "lnb", [128, D], F32)
                xn = sbuf(p0, "xn", [128, D], F32)
                nchunk = max(1, D // 512)
                cw = D // nchunk
                st6 = sbuf(p0, "st6", [128, nchunk, 6], F32)
                mv2 = sbuf(p0, "mv2", [128, 4], F32)
                xs = [sbuf(p0, "xs%d" % i, [128, D], F32) for i in range(2)]
                xs_sc = [pb.new_dma_sc("xs%d" % i) for i in range(2)]
                pb.dma(sp, cst_sc, [], ["memx"],
                       lambda e: e.dma_start(out=memx[:], in_=mem.rearrange("(t p) d -> p t d", p=128)))
                pb.dma(sp, cst_sc, [], ["lng"], lambda e: e.dma_start(out=lng[:], in_=lnm_g))
                pb.dma(sp, cst_sc, [], ["lnb"], lambda e: e.dma_start(out=lnb[:], in_=lnm_b))
                cst_tok = (cst_sc, cst_sc.n)
                for k in ("idf", "idb", "bcol", "wcc", "memx", "lng", "lnb"):
                    pb.deps[k].w = cst_tok

                for t in range(MCH):
                    for ci in range(nchunk):
                        pb.op(dve, ["memx"], [("st6", ci)],
                              lambda e, ci=ci: e.bn_stats(out=st6[:, ci, :], in_=memx[:, t, ci * cw:(ci + 1) * cw]))
                    pb.op(dve, [("st6", ci) for ci in range(nchunk)], ["mv2"],
                          lambda e: e.bn_aggr(out=mv2[:, 0:2], in_=st6[:]))
                    pb.op(dve, ["mv2"], ["mv2r"],
                          lambda e: e.tensor_scalar(out=mv2[:, 2:3], in0=mv2[:, 1:2], scalar1=LN_EPS, scalar2=None,
                                                    op0=ALU.add))
                    pb.op(act, ["mv2r"], ["mv2r"],
                          lambda e: e.activation(out=mv2[:, 2:3], in_=mv2[:, 2:3], func=AF.Sqrt))
                    pb.op(dve, ["mv2r"], ["mv2r"], lambda e: e.reciprocal(out=mv2[:, 2:3], in_=mv2[:, 2:3]))
                    pb.op(dve, ["mv2", "mv2r"], ["mv2n"],
                          lambda e: e.tensor_scalar(out=mv2[:, 3:4], in0=mv2[:, 0:1], scalar1=mv2[:, 2:3], scalar2=-1.0,
                                                    op0=ALU.mult, op1=ALU.mult))
                    pb.op(act, ["memx", "mv2r", "mv2n"], ["xn"],
                          lambda e: e.activation(out=xn[:], in_=memx[:, t, :], func=AF.Identity,
                                                 scale=mv2[:, 2:3], bias=mv2[:, 3:4]))
                    pb.op(dve, ["xn", "lng"], ["xn"], lambda e: e.tensor_tensor(out=xn[:], in0=xn[:], in1=lng[:], op=ALU.mult))
                    pb.op(dve, ["xn", "lnb"], [("memn", t)],
                          lambda e: e.tensor_tensor(out=memn[:, t, :], in0=xn[:], in1=lnb[:], op=ALU.add))
                    for k0 in range(0, KD, 8):
                        kn = min(8, KD - k0)
                        bi, bk = ps_next()
                        pbf = psum[:, bi, :].bitcast(BF16)
                        pb.mm([("memn", t), "idb"], [bk],
                              [lambda e, kk=kk, bi=bi, pbf=pbf: e.transpose(
                                  pbf[:, (kk - k0) * 128:(kk - k0 + 1) * 128], memn[:, t, kk * 128:(kk + 1) * 128], idb[:])
                               for kk in range(k0, k0 + kn)])
                        pb.op(act, [bk], [("memnT", t, k0)],
                              lambda e, pbf=pbf, kn=kn, k0=k0: e.activation(
                                  out=memnT[:, k0:k0 + kn, t * 128:(t + 1) * 128],
                                  in_=pbf[:, 0:kn * 128].rearrange("p (a b) -> p a b", a=kn), func=AF.Copy))
                memnT_keys = [("memnT", t, k0) for t in range(MCH) for k0 in range(0, KD, 8)]
                for cc_ in range(KC_MEM):
                    def cons(g, bi, bk, cc_=cc_):
                        pb.op(act, [bk], [("mkT", cc_)],
                              lambda e: e.activation(out=mkT[:, cc_, :], in_=psum[:, bi, 0:ML], func=AF.Copy))
                    proj_fm(w_mem_kv[:, cc_ * 128:(cc_ + 1) * 128], KD, lambda kc, g: memnT[:, kc, :],
                            memnT_keys, 1, cons, n_cols=ML)
                for cc_ in range(KC_MEM):
                    s = wload(w_mem_kv[:, MEM_DIM + cc_ * 128: MEM_DIM + (cc_ + 1) * 128], KD)
                    for mc in range(MCH):
                        bi, bk = ps_next()
                        pb.mm([("wb", s)] + memnT_keys, [bk],
                              [lambda e, kc=kc, bi=bi, mc=mc: e.matmul(
                                  psum[:, bi, 0:128], memnT[:, kc, mc * 128:(mc + 1) * 128], wbuf[s][:, kc, :],
                                  start=(kc == 0), stop=(kc == KD - 1)) for kc in range(KD)])
                        pb.op(act, [bk], [("mvt", mc, cc_)],
                              lambda e, bi=bi, mc=mc, cc_=cc_: e.activation(
                                  out=mvt[:, mc, cc_ * 128:(cc_ + 1) * 128], in_=psum[:, bi, 0:128], func=AF.Copy))

                for tt in range(NT):
                    b = tt % 2
                    pb.dma(sp, xs_sc[b], [], [("xs", b)],
                           lambda e, b=b, tt=tt: e.dma_start(out=xs[b][:], in_=x[tt * 128:(tt + 1) * 128, :]))
                    for k0 in range(0, KD, 4):
                        kn = min(4, KD - k0)
                        bi, bk = ps_next()
                        pb.mm([("xs", b), "idf"], [bk],
                              [lambda e, kk=kk, bi=bi, b=b: e.transpose(
                                  psum[:, bi, (kk - k0) * 128:(kk - k0 + 1) * 128], xs[b][:, kk * 128:(kk + 1) * 128], idf[:])
                               for kk in range(k0, k0 + kn)])
                        ev = act if (k0 // 4) % 2 == 0 else dve
                        if ev is act:
                            pb.op(act, [bk], [("xT", tt, k0)],
                                  lambda e, bi=bi, kn=kn, k0=k0, tt=tt: e.activation(
                                      out=xT[:, k0:k0 + kn, tt * 128:(tt + 1) * 128],
                                      in_=psum[:, bi, 0:kn * 128].rearrange("p (a b) -> p a b", a=kn), func=AF.Copy))
                        else:
                            pb.op(dve, [bk], [("xT", tt, k0)],
                                  lambda e, bi=bi, kn=kn, k0=k0, tt=tt: e.tensor_copy(
                                      out=xT[:, k0:k0 + kn, tt * 128:(tt + 1) * 128],
                                      in_=psum[:, bi, 0:kn * 128].rearrange("p (a b) -> p a b", a=kn)))
                emit_zero_fill()
                pb.barrier()
            xT_keys = ["xTall"]
            pb.deps["xTall"] = Dep()

            def x_nat(kc, g):
                return xT[:, kc, g * 512:(g + 1) * 512]

            convT = sbuf(s1, "convT", [128, KC_CONV, S], BF16)
            with ExitStack() as p2:
                cct = [sbuf(p2, "cct%d" % i, [128, 512], F32) for i in range(2)]
                ub = [sbuf(p2, "ub%d" % i, [128, S + 2], F32) for i in range(2)]
                yb = [sbuf(p2, "yb%d" % i, [128, S], F32) for i in range(2)]
                for i in range(2):
                    pb.op(dve, [], [("u", i)], lambda e, i=i: e.memset(ub[i][:, 0:2], 0.0))
                cct_ring = Ring(2)
                for f in range(KC_CONV):
                    ui = f % 2
                    colC = (c["cC"] // 128) + f
                    colH = (c["cH"] // 128) + f
                    colB = (c["cB"] // 128) + f
                    slots = {}

                    def cons_c(g, bi, bk):
                        ci = cct_ring.next()
                        slots[g] = ci
                        pb.op(act, [bk, "bcol"], [("cct", ci)],
                              lambda e: e.activation(out=cct[ci][:], in_=psum[:, bi, :], func=AF.Identity,
                                                     bias=bcol[:, colC:colC + 1]))

                    def cons_h(g, bi, bk):
                        ci = slots[g]
                        pb.op(dve, [bk, "bcol", ("cct", ci)], [("u", ui)],
                              lambda e: e.scalar_tensor_tensor(
                                  out=ub[ui][:, 2 + g * 512: 2 + (g + 1) * 512], in0=psum[:, bi, :],
                                  scalar=bcol[:, colH:colH + 1], in1=cct[ci][:], op0=ALU.add, op1=ALU.mult))

                    sC = wload(w_in[:, colC * 128:(colC + 1) * 128], KD)
                    sH = wload(w_in[:, colH * 128:(colH + 1) * 128], KD)
                    for g in range(TGN):
                        for (s, cons) in ((sC, cons_c), (sH, cons_h)):
                            bi, bk = ps_next()
                            pb.mm([("wb", s)] + xT_keys, [bk],
                                  [lambda e, kc=kc, bi=bi, s=s, g=g: e.matmul(
                                      psum[:, bi, :], wbuf[s][:, kc, :], x_nat(kc, g),
                                      start=(kc == 0), stop=(kc == KD - 1)) for kc in range(KD)])
                            cons(g, bi, bk)
                    pb.op(act, [("u", ui), "wcc"], [("y", ui)],
                          lambda e: e.activation(out=yb[ui][:], in_=ub[ui][:, 2:S + 2], func=AF.Copy,
                                                 scale=wcc[:, 2, f:f + 1]))
                    pb.op(dve, [("u", ui), "wcc", ("y", ui)], [("y", ui)],
                          lambda e: e.scalar_tensor_tensor(out=yb[ui][:], in0=ub[ui][:, 1:S + 1], scalar=wcc[:, 1, f:f + 1],
                                                           in1=yb[ui][:], op0=ALU.mult, op1=ALU.add))
                    pb.op(dve, [("u", ui), "wcc", ("y", ui)], [("y", ui)],
                          lambda e: e.scalar_tensor_tensor(out=yb[ui][:], in0=ub[ui][:, 0:S], scalar=wcc[:, 0, f:f + 1],
                                                           in1=yb[ui][:], op0=ALU.mult, op1=ALU.add))

                    def cons_b(g, bi, bk):
                        pb.op(dve, [bk, "bcol", ("y", ui)], [("convT", f, g)],
                              lambda e: e.scalar_tensor_tensor(
                                  out=convT[:, f, g * 512:(g + 1) * 512], in0=psum[:, bi, :],
                                  scalar=bcol[:, colB:colB + 1], in1=yb[ui][:, g * 512:(g + 1) * 512],
                                  op0=ALU.add, op1=ALU.mult))
                    proj_fm(w_in[:, colB * 128:(colB + 1) * 128], KD, x_nat, xT_keys, TGN, cons_b)
                pb.barrier()

            def dbg_dump(t, kcn):
                dbg = nc.dram_tensor("dbg", [kcn, 128, S], BF16, kind="ExternalOutput").ap()
                dsc = pb.new_dma_sc("dbg")
                pb.barrier()
                pb.dma(sp, dsc, [], [], lambda e: e.dma_start(out=dbg.rearrange("k p s -> p k s"), in_=t[:]))
                sp.wait([(dsc, dsc.n)])

            if debug == "conv":
                dbg_dump(convT, KC_CONV)
                return nc, c

            att_scale = 128.0 ** -0.5
            dilT = sbuf(s1, "dilT", [128, KC_DO, S], BF16)
            with ExitStack() as p3:
                qT = [sbuf(p3, "qT%d" % i, [128, S], BF16) for i in range(1)]
                kT = [sbuf(p3, "kT%d" % i, [128, S], BF16) for i in range(1)]
                vT = [sbuf(p3, "vT%d" % i, [128, S], BF16) for i in range(1)]
                Vt = [sbuf(p3, "Vt%d" % i, [128, NT, 128], BF16) for i in range(1)]
                ebs = [sbuf(p3, "ebs%d" % i, [128, 2, 128], F32) for i in range(2)]
                eb_sc = [pb.new_dma_sc("eb%d" % i) for i in range(2)]
                NEB = 3
                Eb = [sbuf(p3, "Eb%d" % i, [128, 2, 128], F32) for i in range(NEB)]
                PT = [sbuf(p3, "PT%d" % i, [128, 2, 128], BF16) for i in range(NEB)]
                e_ring = Ring(NEB)
                OD = sbuf(p3, "OD", [128, 2, S], F32)
                hcount = 0
                for hh in range(HPG):
                    for g in range(NG):
                        window, d = c["PATTERNS"][g]
                        L = S // d
                        assert L % 128 == 0
                        nblk = L // 128
                        h = g * HPG + hh
                        hb = 0
                        ebi = hcount % 2
                        hcount += 1
                        pb.dma(sp, eb_sc[ebi], [], [("eb", ebi)], lambda e: e.dma_start(out=ebs[ebi][:], in_=ebt[h]))

                        def x_perm(kc, tg):
                            view = xT[:, kc, :].rearrange("p (i r) -> p r i", r=d)
                            if L >= 512:
                                r = (tg * 512) // L
                                i0 = (tg * 512) % L
                                return view[:, r, i0:i0 + 512]
                            nr = 512 // L
                            return view[:, tg * nr:(tg + 1) * nr, :]

                        ni = 512 // d
                        for (nm, colbase, dst) in (("q", c["cQ"], qT[hb]), ("k", c["cK"], kT[hb]), ("v", c["cV"], vT[hb])):
                            col = (colbase // 128) + g * HPG + hh

                            def cons(tg, bi, bk):
                                if d == 1:
                                    o_ap = dst[:, tg * 512:(tg + 1) * 512]
                                    i_ap = psum[:, bi, :]
                                else:
                                    o_ap = dst[:, :].rearrange("p (r i) -> p i r", r=d)[:, tg * ni:(tg + 1) * ni, :]
                                    i_ap = psum[:, bi, :].rearrange("p (i r) -> p i r", r=d)
                                pb.op(act, [bk, "bcol"], [(nm, hb)],
                                      lambda e: e.activation(out=o_ap, in_=i_ap, func=AF.Identity, bias=bcol[:, col:col + 1]))
                            proj_fm(w_in[:, col * 128:(col + 1) * 128], KD, x_nat, xT_keys, TGN, cons)
                        for b0 in range(0, NT, 8):
                            bn = min(8, NT - b0)
                            bi, bk = ps_next()
                            pbf = psum[:, bi, :].bitcast(BF16)
                            pb.mm([("v", hb), "idb"], [bk],
                                  [lambda e, bb=bb: e.transpose(pbf[:, (bb - b0) * 128:(bb - b0 + 1) * 128],
                                                                vT[hb][:, bb * 128:(bb + 1) * 128], idb[:])
                                   for bb in range(b0, b0 + bn)])
                            pb.op(dve, [bk], [("Vt", hb, b0)],
                                  lambda e: e.tensor_copy(out=Vt[hb][:, b0:b0 + bn, :],
                                                          in_=pbf[:, 0:bn * 128].rearrange("p (a b) -> p a b", a=bn)))
                        qk_keys = [("q", hb), ("k", hb)]
                        vt_keys = [("Vt", hb, b0) for b0 in range(0, NT, 8)]

                        def emit_s(bidx):
                            n = bidx % nblk
                            nk = 2 if n > 0 else 1
                            pbase = bidx * 128
                            bi, bk = ps_next()
                            fns = [lambda e: e.matmul(psum[:, bi, 0:128], kT[hb][:, pbase:pbase + 128],
                                                      qT[hb][:, pbase:pbase + 128], start=True, stop=True)]
                            if nk == 2:
                                fns.append(lambda e: e.matmul(psum[:, bi, 128:256], kT[hb][:, pbase - 128:pbase],
                                                              qT[hb][:, pbase:pbase + 128], start=True, stop=True))
                            pb.mm(qk_keys, [bk], fns)
                            ei = e_ring.next()
                            pb.op(act, [bk], [("Eb", ei)],
                                  lambda e: e.activation(out=Eb[ei][:, 0:nk, :],
                                                         in_=psum[:, bi, 0:nk * 128].rearrange("p (a b) -> p a b", a=nk),
                                                         func=AF.Exp, scale=att_scale))
                            pb.op(dve, [("Eb", ei), ("eb", ebi)], [("PT", ei)],
                                  lambda e: e.tensor_tensor(out=PT[ei][:, 0:nk, :], in0=Eb[ei][:, 0:nk, :],
                                                            in1=ebs[ebi][:, 0:nk, :], op=ALU.mult))
                            return (bidx, nk, ei)

                        def emit_pv(st):
                            bidx, nk, ei = st
                            r = bidx // nblk
                            n = bidx % nblk
                            bo, bko = ps_next()
                            fns = [lambda e: e.matmul(psum[:, bo, 0:128], Vt[hb][:, bidx, :], PT[ei][:, 0, :],
                                                      start=True, stop=(nk == 1))]
                            if nk == 2:
                                fns.append(lambda e: e.matmul(psum[:, bo, 0:128], Vt[hb][:, bidx - 1, :], PT[ei][:, 1, :],
                                                              start=False, stop=True))
                            fns.append(lambda e: e.matmul(psum[:, bo, 128:256], ones_b[:], PT[ei][:, 0, :],
                                                          start=True, stop=(nk == 1)))
                            if nk == 2:
                                fns.append(lambda e: e.matmul(psum[:, bo, 128:256], ones_b[:], PT[ei][:, 1, :],
                                                              start=False, stop=True))
                            pb.mm(vt_keys + [("PT", ei), "ones_b"], [bko], fns)
                            t0 = r + d * n * 128
                            od_ap = OD[:, :, t0:min(S, t0 + d * 128):d]
                            src = psum[:, bo, 0:256].rearrange("p (a b) -> p a b", a=2)
                            if g == 0:
                                pb.op(act, [bko], ["OD"], lambda e: e.activation(out=od_ap, in_=src, func=AF.Copy))
                            else:
                                pb.op(dve, [bko, "OD"], ["OD"],
                                      lambda e: e.tensor_tensor(out=od_ap, in0=src, in1=od_ap, op=ALU.add))

                        pend = None
                        for bidx in range(NT):
                            st = emit_s(bidx)
                            if pend is not None:
                                emit_pv(pend)
                            pend = st
                        emit_pv(pend)
                    pb.op(dve, ["OD"], ["OD"], lambda e: e.reciprocal(out=OD[:, 1, :], in_=OD[:, 1, :]))
                    pb.op(dve, ["OD"], [("dilT", hh)],
                          lambda e: e.tensor_tensor(out=dilT[:, hh, :], in0=OD[:, 0, :], in1=OD[:, 1, :], op=ALU.mult))
                pb.barrier()

            if debug == "dil":
                dbg_dump(dilT, KC_DO)
                return nc, c

            mem_scale = float(c["MEM_HD"]) ** -0.5
            memT = sbuf(s1, "memT", [128, KC_MEM, S], BF16)
            with ExitStack() as p4:
                mqT = [sbuf(p4, "mqT%d" % i, [128, HC, S], BF16) for i in range(2)]
                PTm = [sbuf(p4, "PTm%d" % i, [128, MCH, 512], BF16) for i in range(2)]
                rDm = [sbuf(p4, "rDm%d" % i, [128, 512], F32) for i in range(2)]
                pt_ring = Ring(2)
                for mh in range(c["MEM_HEADS"]):
                    hb = mh % 2
                    for hc in range(HC):
                        col = (c["cMQ"] // 128) + mh * HC + hc

                        def cons(tg, bi, bk):
                            pb.op(act, [bk, "bcol"], [("mq", hb, hc, tg)],
                                  lambda e: e.activation(out=mqT[hb][:, hc, tg * 512:(tg + 1) * 512], in_=psum[:, bi, :],
                                                         func=AF.Identity, bias=bcol[:, col:col + 1]))
                        proj_fm(w_in[:, col * 128:(col + 1) * 128], KD, x_nat, xT_keys, TGN, cons)
                    for tg in range(TGN):
                        pi = pt_ring.next()
                        for mc in range(MCH):
                            bi, bk = ps_next()
                            pb.mm([("mq", hb, hc, tg) for hc in range(HC)], [bk],
                                  [lambda e, hc=hc: e.matmul(psum[:, bi, :], mkT[:, mh * HC + hc, mc * 128:(mc + 1) * 128],
                                                             mqT[hb][:, hc, tg * 512:(tg + 1) * 512],
                                                             start=(hc == 0), stop=(hc == HC - 1)) for hc in range(HC)])
                            pb.op(act, [bk], [("PTm", pi, mc)],
                                  lambda e: e.activation(out=PTm[pi][:, mc, :], in_=psum[:, bi, :], func=AF.Exp,
                                                         scale=mem_scale))
                        ptk = [("PTm", pi, mc) for mc in range(MCH)]
                        bd, bkd = ps_next()
                        pb.mm(ptk + ["ones_b"], [bkd],
                              [lambda e, mc=mc: e.matmul(psum[:, bd, :], ones_b[:], PTm[pi][:, mc, :],
                                                         start=(mc == 0), stop=(mc == MCH - 1)) for mc in range(MCH)])
                        pb.op(dve, [bkd], [("rDm", pi)], lambda e: e.reciprocal(out=rDm[pi][:], in_=psum[:, bd, :]))
                        for oc in range(HC):
                            bo, bko = ps_next()
                            pb.mm(ptk, [bko],
                                  [lambda e, mc=mc: e.matmul(psum[:, bo, :],
                                                             mvt[:, mc, mh * c["MEM_HD"] + oc * 128: mh * c["MEM_HD"] + (oc + 1) * 128],
                                                             PTm[pi][:, mc, :], start=(mc == 0), stop=(mc == MCH - 1))
                                   for mc in range(MCH)])
                            pb.op(dve, [bko, ("rDm", pi)], [("memT", mh * HC + oc, tg)],
                                  lambda e: e.tensor_tensor(out=memT[:, mh * HC + oc, tg * 512:(tg + 1) * 512],
                                                            in0=psum[:, bo, :], in1=rDm[pi][:], op=ALU.mult))
                pb.barrier()

            if debug == "mem":
                dbg_dump(memT, KC_MEM)
                return nc, c

            with ExitStack() as p5:
                sg = [sbuf(p5, "sg%d" % i, [128, S], F32) for i in range(1)]
                macc = [sbuf(p5, "macc%d" % i, [128, S], F32) for i in range(1)]
                tmpm = [sbuf(p5, "tmpm%d" % i, [128, 512], F32) for i in range(2)]
                mrg = [sbuf(p5, "mrg%d" % i, [128, S], BF16) for i in range(1)]
                mrg_sc = [pb.new_dma_sc("mrg%d" % i) for i in range(1)]
                tm_ring = Ring(2)
                branches = ((w_conv_out, KC_CONV, convT), (w_dil_out, KC_DO, dilT), (w_mem_out, KC_MEM, memT))
                for j in range(KD):
                    jb = 0
                    for br, (wout, kcn, actT) in enumerate(branches):
                        gcol = (c["cG"] // 128) + br * KD + j

                        def cons_g(tg, bi, bk):
                            pb.op(act, [bk, "bcol"], [("sg", jb, tg)],
                                  lambda e: e.activation(out=sg[jb][:, tg * 512:(tg + 1) * 512], in_=psum[:, bi, :],
                                                         func=AF.Sigmoid, bias=bcol[:, gcol:gcol + 1]))
                        proj_fm(w_in[:, gcol * 128:(gcol + 1) * 128], KD, x_nat, xT_keys, TGN, cons_g)

                        def cons_y(tg, bi, bk):
                            sl = slice(tg * 512, (tg + 1) * 512)
                            if br == 0:
                                pb.op(dve, [bk, ("sg", jb, tg)], [("macc", jb, tg)],
                                      lambda e: e.tensor_tensor(out=macc[jb][:, sl], in0=psum[:, bi, :], in1=sg[jb][:, sl],
                                                                op=ALU.mult))
                            else:
                                ti = tm_ring.next()
                                pb.op(dve, [bk, ("sg", jb, tg)], [("tmpm", ti)],
                                      lambda e: e.tensor_tensor(out=tmpm[ti][:], in0=psum[:, bi, :], in1=sg[jb][:, sl],
                                                                op=ALU.mult))
                                if br == 1:
                                    pb.op(dve, [("tmpm", ti), ("macc", jb, tg)], [("macc", jb, tg)],
                                          lambda e: e.tensor_tensor(out=macc[jb][:, sl], in0=tmpm[ti][:], in1=macc[jb][:, sl],
                                                                    op=ALU.add))
                                else:
                                    pb.op(dve, [("tmpm", ti), ("macc", jb, tg)], [("mrg", jb)],
                                          lambda e: e.tensor_tensor(out=mrg[jb][:, sl], in0=tmpm[ti][:], in1=macc[jb][:, sl],
                                                                    op=ALU.add))
                        proj_fm(wout[:, j * 128:(j + 1) * 128], kcn, lambda kc, tg: actT[:, kc, tg * 512:(tg + 1) * 512],
                                [], TGN, cons_y)
                    pb.dma(sp, mrg_sc[jb], [("mrg", jb)], [("mergedT", j)],
                           lambda e: e.dma_start(out=mergedT[j], in_=mrg[jb][:]))
                pb.barrier()

            if debug == "merged":
                sp.wait([(sc_, sc_.n) for sc_ in mrg_sc])
                return nc, c

        rt = ExitStack()
        top.enter_context(rt)
        ones_f = sbuf(rt, "ones_f", [128, 128], F32)
        trif = sbuf(rt, "trif", [128, 128], F32)
        OH1 = sbuf(rt, "OH1", [128, NT, NE], F32)
        OH2 = sbuf(rt, "OH2", [128, NT, NE], F32)
        OHs = sbuf(rt, "OHs", [128, NT, NE], F32)
        w12 = sbuf(rt, "w12", [128, NT, 2], F32)
        slf = sbuf(rt, "slf", [128, NT, 2], F32)
        sli = sbuf(rt, "sli", [128, NT, 2], I32)
        soff = sbuf(rt, "soff", [128, NE], F32)
        toff = sbuf(rt, "toff", [128, NE], F32)
        tei = sbuf(rt, "tei", [128, NTILE], I32)
        iop = sbuf(rt, "iop", [128, 1], F32)
        gln = sbuf(rt, "gln", [128, D], F32)
        bln = sbuf(rt, "bln", [128, D], F32)
        pb.op(dve, [], ["ones_f"], lambda e: e.memset(ones_f[:], 1.0))
        c2_sc = pb.new_dma_sc("c2")
        pb.dma(sp, c2_sc, [], ["trif"], lambda e: e.dma_start(out=trif[:], in_=tri))
        pb.dma(sp, c2_sc, [], ["iop"], lambda e: e.dma_start(out=iop[:], in_=iota_p))

        def ln_rows(src_ap, dst_ap, src_key, dst_key, st6, mv4, g_ap, b_ap, gb_keys, mul_eng=None):
            mul_eng = mul_eng or pool
            nch = max(1, D // 512)
            cw_ = D // nch
            for ci in range(nch):
                pb.op(dve, [src_key], [("st6", ci)],
                      lambda e: e.bn_stats(out=st6[:, ci, :], in_=src_ap[:, ci * cw_:(ci + 1) * cw_]))
            pb.op(dve, [("st6", ci) for ci in range(nch)], ["mv4"], lambda e: e.bn_aggr(out=mv4[:, 0:2], in_=st6[:]))
            pb.op(dve, ["mv4"], ["mv4r"],
                  lambda e: e.tensor_scalar(out=mv4[:, 2:3], in0=mv4[:, 1:2], scalar1=LN_EPS, scalar2=None, op0=ALU.add))
            pb.op(act, ["mv4r"], ["mv4r"], lambda e: e.activation(out=mv4[:, 2:3], in_=mv4[:, 2:3], func=AF.Ln))
            pb.op(act, ["mv4r"], ["mv4r"], lambda e: e.activation(out=mv4[:, 2:3], in_=mv4[:, 2:3], func=AF.Exp, scale=-0.5))
            pb.op(dve, ["mv4", "mv4r"], ["mv4n"],
                  lambda e: e.tensor_scalar(out=mv4[:, 3:4], in0=mv4[:, 0:1], scalar1=mv4[:, 2:3], scalar2=-1.0,
                                            op0=ALU.mult, op1=ALU.mult))
            pb.op(act, [src_key, "mv4r", "mv4n"], [src_key],
                  lambda e: e.activation(out=src_ap, in_=src_ap, func=AF.Identity, scale=mv4[:, 2:3], bias=mv4[:, 3:4]))
            pb.op(mul_eng, [src_key] + gb_keys, [src_key],
                  lambda e: e.tensor_tensor(out=src_ap, in0=src_ap, in1=g_ap, op=ALU.mult))
            pb.op(dve, [src_key] + gb_keys, [dst_key],
                  lambda e: e.tensor_tensor(out=dst_ap, in0=src_ap, in1=b_ap, op=ALU.add))

        with ExitStack() as p6:
            wo = sbuf(p6, "wo", [128, KD, D], BF16)
            NQ = max(1, D // 512)
            qw = D // NQ
            wo_sc = [pb.new_dma_sc("wo%d" % n) for n in range(NQ)]
            for n in range(NQ):
                pb.dma(pool, wo_sc[n], [], [("wo", n)],
                       lambda e: e.dma_start(out=wo[:, :, n * qw:(n + 1) * qw],
                                             in_=w_o[:, n * qw:(n + 1) * qw].rearrange("(kc p) n -> p kc n", p=128)))
            pb.dma(sp, c2_sc, [], ["gln"], lambda e: e.dma_start(out=gln[:], in_=ln1_g))
            pb.dma(sp, c2_sc, [], ["bln"], lambda e: e.dma_start(out=bln[:], in_=ln1_b))
            wr = sbuf(p6, "wr", [128, KD, NR], F32)
            brt = sbuf(p6, "brt", [128, NR], F32)
            pb.dma(sp, c2_sc, [], ["wr"], lambda e: e.dma_start(out=wr[:], in_=w_r.rearrange("(kc p) n -> p kc n", p=128)))
            pb.dma(sp, c2_sc, [], ["brt"], lambda e: e.dma_start(out=brt[:], in_=b_r))
            c2_tok = (c2_sc, c2_sc.n)
            for k in ("trif", "iop", "gln", "bln", "wr", "brt"):
                pb.deps[k].w = c2_tok
            GT = min(2, NT)
            mTg = [sbuf(p6, "mTg%d" % i, [128, KD, GT * 128], BF16) for i in range(2)]
            mT_sc = [pb.new_dma_sc("mT%d" % i) for i in range(2)]
            xres = [sbuf(p6, "xres%d" % i, [128, D], F32) for i in range(2)]
            xr_sc = [pb.new_dma_sc("xr%d" % i) for i in range(2)]
            NB6 = 3
            h1 = [sbuf(p6, "h1_%d" % i, [128, D], F32) for i in range(NB6)]
            x1o = h1
            x1o_sc = [pb.new_dma_sc("x1o%d" % i) for i in range(NB6)]
            x1b = [sbuf(p6, "x1b%d" % i, [128, D], BF16) for i in range(NB6)]
            x1b_sc = [pb.new_dma_sc("x1b%d" % i) for i in range(NB6)]
            x1T = sbuf(p6, "x1T", [128, KD, 128], F32)
            st6b = sbuf(p6, "st6b", [128, max(1, D // 512), 6], F32)
            mv4 = sbuf(p6, "mv4", [128, 4], F32)
            G_, E_ = c["G"], c["E"]
            lga = sbuf(p6, "lga", [128, NT, NR], F32)
            gsh = sbuf(p6, "gsh", [128, NT, G_], F32)
            ohg = sbuf(p6, "ohg", [128, NT, G_], F32)
            elog = sbuf(p6, "elog", [128, NT, E_], F32)
            elog2 = sbuf(p6, "elog2", [128, NT, E_], F32)
            tmpE = sbuf(p6, "tmpE", [128, NT, E_], F32)
            oh1l = sbuf(p6, "oh1l", [128, NT, E_], F32)
            oh2l = sbuf(p6, "oh2l", [128, NT, E_], F32)
            sm = sbuf(p6, "sm", [128, 10, NT], F32)
            def p6_loads(tt):
                b = tt % 2
                gi = tt // GT
                gb = gi % 2
                if tt % GT == 0:
                    pb.dma(sp, mT_sc[gb], [("mergedT", j) for j in range(KD)], [("mTg", gb)],
                           lambda e: e.dma_start(out=mTg[gb][:], in_=mergedT[:, :, gi * GT * 128:(gi + 1) * GT * 128]
                                                 .rearrange("j p t -> p j t")))
                pb.dma(sp, xr_sc[b], [], [("xres", b)], lambda e: e.dma_start(out=xres[b][:], in_=x[tt * 128:(tt + 1) * 128, :]))

            def p6_A(tt):
                b = tt % 2
                hb3 = tt % NB6
                gi = tt // GT
                gb = gi % 2
                if tt + 1 < NT:
                    p6_loads(tt + 1)
                tl = (tt % GT) * 128
                for n in range(NQ):
                    bi, bk = ps_next()
                    pb.mm([("mTg", gb), ("wo", n)], [bk],
                          [lambda e, j=j: e.matmul(psum[:, bi, 0:qw], mTg[gb][:, j, tl:tl + 128], wo[:, j, n * qw:(n + 1) * qw],
                                                   start=(j == 0), stop=(j == KD - 1)) for j in range(KD)])
                    pb.op(dve, [bk, ("xres", b)], [("h1", hb3)],
                          lambda e: e.scalar_tensor_tensor(out=h1[hb3][:, n * qw:(n + 1) * qw], in0=xres[b][:, n * qw:(n + 1) * qw],
                                                           scalar=ALPHA, in1=psum[:, bi, 0:qw], op0=ALU.mult, op1=ALU.add))

            def p6_A2(tt):
                hb3 = tt % NB6
                ln_rows(h1[hb3][:], h1[hb3][:], ("h1", hb3), ("h1", hb3), st6b, mv4, gln[:], bln[:], ["gln", "bln"])
                pb.dma(sp, x1o_sc[hb3], [("h1", hb3)], [("X1F", tt)],
                       lambda e: e.dma_start(out=X1F[tt * 128:(tt + 1) * 128, :], in_=x1o[hb3][:]))
                pb.op(act, [("h1", hb3)], [("x1b", hb3)], lambda e: e.activation(out=x1b[hb3][:], in_=x1o[hb3][:], func=AF.Copy))
                pb.dma(sp, x1b_sc[hb3], [("x1b", hb3)], [("X1B", tt)],
                       lambda e: e.dma_start(out=X1B[tt * 128:(tt + 1) * 128, :], in_=x1b[hb3][:]))

            def p6_B(tt):
                hb3 = tt % NB6
                for k0 in range(0, KD, 4):
                    kn = min(4, KD - k0)
                    bi, bk = ps_next()
                    pb.mm([("h1", hb3), "idf"], [bk],
                          [lambda e, kk=kk: e.transpose(psum[:, bi, (kk - k0) * 128:(kk - k0 + 1) * 128],
                                                        x1o[hb3][:, kk * 128:(kk + 1) * 128], idf[:]) for kk in range(k0, k0 + kn)])
                    pb.op(act, [bk], [("x1T", k0)],
                          lambda e: e.activation(out=x1T[:, k0:k0 + kn, :],
                                                 in_=psum[:, bi, 0:kn * 128].rearrange("p (a b) -> p a b", a=kn), func=AF.Copy))
                bi, bk = ps_next()
                pb.mm([("x1T", k0) for k0 in range(0, KD, 4)] + ["wr"], [bk],
                      [lambda e, kc=kc: e.matmul(psum[:, bi, 0:NR], x1T[:, kc, :], wr[:, kc, :],
                                                 start=(kc == 0), stop=(kc == KD - 1)) for kc in range(KD)])
                pb.op(dve, [bk, "brt"], [("lga", tt)],
                      lambda e: e.tensor_tensor(out=lga[:, tt, :], in0=psum[:, bi, 0:NR], in1=brt[:], op=ALU.add))

            p6_loads(0)
            p6_A(0)
            p6_A2(0)
            if NT > 1:
                p6_A(1)
                p6_A2(1)
            for tt in range(NT):
                if tt + 2 < NT:
                    p6_A(tt + 2)
                p6_B(tt)
                if tt + 2 < NT:
                    p6_A2(tt + 2)

            lk = [("lga", tt) for tt in range(NT)]
            glog = lga[:, :, 0:G_]

            def bc(ap2, n):
                return ap2.unsqueeze(2).to_broadcast([128, NT, n])

            pb.op(dve, lk, ["gmax"], lambda e: e.reduce_max(out=sm[:, 0, :], in_=glog, axis=AX.X))
            pb.op(dve, lk + ["gmax"], ["gsh"], lambda e: e.tensor_tensor(out=gsh[:], in0=glog, in1=bc(sm[:, 0, :], G_), op=ALU.subtract))
            pb.op(act, ["gsh"], ["ohg"], lambda e: e.activation(out=ohg[:], in_=gsh[:], func=AF.Exp))
            pb.op(dve, ["ohg"], ["gsum"], lambda e: e.reduce_sum(out=sm[:, 1, :], in_=ohg[:], axis=AX.X))
            pb.op(dve, ["gsum"], ["gw"], lambda e: e.reciprocal(out=sm[:, 2, :], in_=sm[:, 1, :]))
            pb.op(dve, ["gsh", "ohg", "gsum"], ["ohg"],
                  lambda e: e.tensor_scalar(out=ohg[:], in0=gsh[:], scalar1=0.0, scalar2=None, op0=ALU.is_equal))
            for g in range(G_):
                src = lga[:, :, G_ + g * E_: G_ + (g + 1) * E_]
                dst = elog if g == 0 else tmpE
                pb.op(dve, lk + ["ohg", "tmpE", "elog"], ["tmpE" if g else "elog"],
                      lambda e: e.tensor_tensor(out=dst[:], in0=src, in1=bc(ohg[:, :, g], E_), op=ALU.mult))
                if g:
                    pb.op(dve, ["tmpE", "elog"], ["elog"], lambda e: e.tensor_tensor(out=elog[:], in0=elog[:], in1=tmpE[:], op=ALU.add))
            pb.op(dve, ["elog"], ["v1"], lambda e: e.reduce_max(out=sm[:, 3, :], in_=elog[:], axis=AX.X))
            pb.op(dve, ["elog", "v1"], ["oh1l"], lambda e: e.tensor_tensor(out=oh1l[:], in0=elog[:], in1=bc(sm[:, 3, :], E_), op=ALU.is_equal))
            pb.op(dve, ["elog", "oh1l"], ["elog2"],
                  lambda e: e.scalar_tensor_tensor(out=elog2[:], in0=oh1l[:], scalar=-1.0e30, in1=elog[:], op0=ALU.mult, op1=ALU.add))
            pb.op(dve, ["elog2"], ["v2"], lambda e: e.reduce_max(out=sm[:, 4, :], in_=elog2[:], axis=AX.X))
            pb.op(dve, ["elog2", "v2"], ["oh2l"], lambda e: e.tensor_tensor(out=oh2l[:], in0=elog2[:], in1=bc(sm[:, 4, :], E_), op=ALU.is_equal))
            pb.op(dve, ["v1", "v2"], ["e21"], lambda e: e.tensor_tensor(out=sm[:, 5, :], in0=sm[:, 4, :], in1=sm[:, 3, :], op=ALU.subtract))
            pb.op(act, ["e21"], ["e21"], lambda e: e.activation(out=sm[:, 5, :], in_=sm[:, 5, :], func=AF.Exp))
            pb.op(dve, ["e21"], ["den"], lambda e: e.tensor_scalar(out=sm[:, 6, :], in0=sm[:, 5, :], scalar1=1.0, scalar2=None, op0=ALU.add))
            pb.op(dve, ["den"], ["w1"], lambda e: e.reciprocal(out=sm[:, 7, :], in_=sm[:, 6, :]))
            pb.op(dve, ["w1", "e21"], ["w2"], lambda e: e.tensor_tensor(out=sm[:, 8, :], in0=sm[:, 7, :], in1=sm[:, 5, :], op=ALU.mult))
            for k in range(2):
                pb.op(dve, ["w1", "w2", "gw"], [("w12", k)],
                      lambda e: e.tensor_tensor(out=w12[:, :, k], in0=sm[:, 7 + k, :], in1=sm[:, 2, :], op=ALU.mult))
            for g in range(G_):
                pb.op(dve, ["oh1l", "ohg"], [("OH1g", g)],
                      lambda e: e.tensor_tensor(out=OH1[:, :, g * E_:(g + 1) * E_], in0=oh1l[:], in1=bc(ohg[:, :, g], E_), op=ALU.mult))
                pb.op(dve, ["oh2l", "ohg"], [("OH2g", g)],
                      lambda e: e.tensor_tensor(out=OH2[:, :, g * E_:(g + 1) * E_], in0=oh2l[:], in1=bc(ohg[:, :, g], E_), op=ALU.mult))
            pb.op(dve, [("OH1g", g) for g in range(G_)] + [("OH2g", g) for g in range(G_)], ["OHs"],
                  lambda e: e.tensor_tensor(out=OHs[:], in0=OH1[:], in1=OH2[:], op=ALU.add))
            pb.barrier()

        if debug == "x1":
            sp.wait([(sc_, sc_.n) for sc_ in x1o_sc])
            return nc, c

        scat_sc = [pb.new_dma_sc("scat%d" % i) for i in range(2)]
        p67 = ExitStack()
        rt.enter_context(p67)
        Wg = [sbuf(p67, "Wg%d" % i, [128, KD, F], BF16) for i in range(2)]
        Wu = [sbuf(p67, "Wu%d" % i, [128, KD, F], BF16) for i in range(2)]
        Wd = [sbuf(p67, "Wd%d" % i, [128, FC, D], BF16) for i in range(2)]
        w_sc = [SemC(pb.newsem("d_W%d" % i)) for i in range(2)]
        bnd_reg = nc.gpsimd.alloc_register("wbound")
        nc.gpsimd.reg_mov(bnd_reg, NE * 128 - 1)

        def p7_W(j):
            b = j % 2
            pb.dma_group(pool, w_sc[b], ["tei"], [("W", b)],
                         [lambda e, dst=dst, srcw=srcw: e.indirect_dma_start(
                             out=dst[:].rearrange("p a b -> p (a b)"), out_offset=None, in_=srcw,
                             in_offset=bass.IndirectOffsetOnAxis(ap=tei[:, j:j + 1], axis=0),
                             bounds_check=bnd_reg, oob_is_err=False)
                          for (dst, srcw) in ((Wg[b], w_gate), (Wu[b], w_up), (Wd[b], w_down))])

        with ExitStack() as p6b:
            cntB = sbuf(p6b, "cntB", [128, NE], F32)
            ntl = sbuf(p6b, "ntl", [128, NE], F32)
            scb = [sbuf(p6b, "scb%d" % i, [128, NE], F32) for i in range(2)]
            tef = sbuf(p6b, "tef", [128, NTILE], F32)
            iot = sbuf(p6b, "iot", [128, NTILE], F32)
            base = sbuf(p6b, "base", [128, NE], F32)
            prod = sbuf(p6b, "prod", [128, NE], F32)
            xb = [sbuf(p6b, "xb%d" % i, [128, D], BF16) for i in range(2)]
            xb_sc = [pb.new_dma_sc("xb%d" % i) for i in range(2)]
            io_sc = pb.new_dma_sc("iot")
            pb.dma(sp, io_sc, [], ["iot"], lambda e: e.dma_start(out=iot[:], in_=iota_t))
            oh_keys = [("OHs", tt) for tt in range(NT)]
            bi, bk = ps_next()
            pb.mm(oh_keys + ["ones_f"], [bk],
                  [lambda e, tt=tt: e.matmul(psum[:, bi, 0:NE], ones_f[:], OHs[:, tt, :], start=(tt == 0), stop=(tt == NT - 1))
                   for tt in range(NT)])
            pb.op(dve, [bk], ["cntB"], lambda e: e.tensor_copy(out=cntB[:], in_=psum[:, bi, 0:NE]))
            MAXT = S // TS
            pb.op(dve, ["cntB"], ["ntl"], lambda e: e.tensor_scalar(out=ntl[:], in0=cntB[:], scalar1=0.0, scalar2=None, op0=ALU.is_gt))
            for m in range(1, MAXT):
                pb.op(dve, ["cntB", "ntl"], ["ntl"],
                      lambda e: e.scalar_tensor_tensor(out=ntl[:], in0=cntB[:], scalar=float(m * TS), in1=ntl[:],
                                                       op0=ALU.is_gt, op1=ALU.add))
            cur, curk = ntl, "ntl"
            sh = 1
            ib = 0
            while sh < NE:
                nxt, nxtk = scb[ib], ("scb", ib)
                pb.op(dve, [curk], [nxtk], lambda e: e.tensor_copy(out=nxt[:, 0:sh], in_=cur[:, 0:sh]))
                pb.op(dve, [curk, nxtk], [nxtk],
                      lambda e: e.tensor_tensor(out=nxt[:, sh:NE], in0=cur[:, sh:NE], in1=cur[:, 0:NE - sh], op=ALU.add))
                cur, curk = nxt, nxtk
                ib = 1 - ib
                sh *= 2
            pb.op(dve, [curk, "ntl"], ["toff"], lambda e: e.tensor_tensor(out=toff[:], in0=cur[:], in1=ntl[:], op=ALU.subtract))
            pb.op(dve, ["toff"], ["soff"], lambda e: e.tensor_scalar(out=soff[:], in0=toff[:], scalar1=float(TS), scalar2=None, op0=ALU.mult))
            pb.op(dve, [], ["tef"], lambda e: e.memset(tef[:], -1.0))
            for ex in range(NE):
                pb.op(dve, ["toff", "iot", "tef"], ["tef"],
                      lambda e: e.scalar_tensor_tensor(out=tef[:], in0=iot[:], scalar=toff[:, ex:ex + 1], in1=tef[:],
                                                       op0=ALU.is_ge, op1=ALU.add))
            padf = sbuf(p6b, "padf", [128, NTILE], F32)
            pb.op(dve, [curk, "iot"], ["padf"],
                  lambda e: e.tensor_scalar(out=padf[:], in0=iot[:], scalar1=cur[:, NE - 1:NE], scalar2=float(NE),
                                            op0=ALU.is_ge, op1=ALU.mult))
            pb.op(dve, ["padf", "tef"], ["tef"], lambda e: e.tensor_tensor(out=tef[:], in0=tef[:], in1=padf[:], op=ALU.add))
            pb.op(dve, ["tef", "iop"], ["tef"],
                  lambda e: e.tensor_scalar(out=tef[:], in0=tef[:], scalar1=128.0, scalar2=iop[:, 0:1], op0=ALU.mult, op1=ALU.add))
            pb.op(dve, ["tef"], ["tei"], lambda e: e.tensor_copy(out=tei[:], in_=tef[:]))
            p7_W(0)
            p7_W(1)
            for tt in range(NT):
                b = tt % 2
                bi, bk = ps_next()
                fns = [lambda e: e.matmul(psum[:, bi, 0:NE], trif[:], OHs[:, tt, :], start=True, stop=(tt == 0))]
                for t2 in range(tt):
                    fns.append(lambda e, t2=t2: e.matmul(psum[:, bi, 0:NE], ones_f[:], OHs[:, t2, :], start=False, stop=(t2 == tt - 1)))
                pb.mm(oh_keys + ["trif", "ones_f"], [bk], fns)
                pb.op(dve, [bk, "soff"], ["base"], lambda e: e.tensor_tensor(out=base[:], in0=psum[:, bi, 0:NE], in1=soff[:], op=ALU.add))
                for k, OHk, ohn in ((0, OH1, "OH1"), (1, OH2, "OH2")):
                    pb.op(dve, ["base", (ohn, tt), "prod"], ["prod"],
                          lambda e: e.tensor_tensor(out=prod[:], in0=base[:], in1=OHk[:, tt, :], op=ALU.mult))
                    pb.op(dve, ["prod"], [("slf", tt, k)], lambda e: e.reduce_sum(out=slf[:, tt, k:k + 1], in_=prod[:], axis=AX.X))
                pb.op(dve, [("slf", tt, 0), ("slf", tt, 1)], [("sli", tt)], lambda e: e.tensor_copy(out=sli[:, tt, :], in_=slf[:, tt, :]))
                pb.dma(sp, xb_sc[b], [], [("xb", b)], lambda e: e.dma_start(out=xb[b][:], in_=X1B[tt * 128:(tt + 1) * 128, :]))
                pool.wait([zf_box[0]])
                pb.dma_group(pool, scat_sc[b], [("xb", b), ("sli", tt)], [],
                             [lambda e, k=k: e.indirect_dma_start(
                                 out=Xs, out_offset=bass.IndirectOffsetOnAxis(ap=sli[:, tt, k:k + 1], axis=0),
                                 in_=xb[b][:], in_offset=None) for k in range(2)])
            pb.barrier(keep=[("W", 0), ("W", 1), "tei"])

        if debug == "route":
            dsl = nc.dram_tensor("dsl", [128, NT, 2], I32, kind="ExternalOutput").ap()
            dw = nc.dram_tensor("dw", [128, NT, 2], F32, kind="ExternalOutput").ap()
            dte = nc.dram_tensor("dte", [128, NTILE], I32, kind="ExternalOutput").ap()
            dsc = pb.new_dma_sc("dbg")
            pb.dma(sp, dsc, [], [], lambda e: e.dma_start(out=dsl, in_=sli[:]))
            pb.dma(sp, dsc, [], [], lambda e: e.dma_start(out=dw, in_=w12[:]))
            pb.dma(sp, dsc, [], [], lambda e: e.dma_start(out=dte, in_=tei[:]))
            sp.wait([(dsc, dsc.n)])
            return nc, c

        ys_scs = []
        with ExitStack() as p7:
            xst = [sbuf(p7, "xst%d" % i, [128, SUB, D], BF16) for i in range(2)]
            xst_sc = [pb.new_dma_sc("xst%d" % i) for i in range(2)]
            XsT = [sbuf(p7, "XsT%d" % i, [128, KD, TS], BF16) for i in range(2)]
            hT = [sbuf(p7, "hT%d" % i, [128, FC, TS], BF16) for i in range(2)]
            sa = [sbuf(p7, "sa%d" % i, [128, TS], F32) for i in range(2)]
            ysb = [sbuf(p7, "ysb%d" % i, [128, D], F32) for i in range(2)]
            ys_sc = [pb.new_dma_sc("ys%d" % i) for i in range(2)]
            ys_scs = ys_sc
            sa_ring = Ring(2)
            ys_ring = Ring(2)
            NQ = max(1, D // 512)
            qw = D // NQ
            ev_alt = [0]
            def p7_xload(j):
                b = j % 2
                pb.dma(sp, xst_sc[b], [], [("xst", b)],
                       lambda e: e.dma_start(out=xst[b][:], in_=Xs[j * TS:(j + 1) * TS, :].rearrange("(s p) d -> p s d", p=128)))

            def p7_T(j):
                b = j % 2
                if j + 1 < NTILE:
                    p7_xload(j + 1)
                for sub in range(SUB):
                    for k0 in range(0, KD, 8):
                        kn = min(8, KD - k0)
                        bi, bk = ps_next()
                        pbf = psum[:, bi, :].bitcast(BF16)
                        pb.mm([("xst", b), "idb"], [bk],
                              [lambda e, kk=kk: e.transpose(pbf[:, (kk - k0) * 128:(kk - k0 + 1) * 128],
                                                            xst[b][:, sub, kk * 128:(kk + 1) * 128], idb[:]) for kk in range(k0, k0 + kn)])
                        ev_alt[0] ^= 1
                        src_ap = pbf[:, 0:kn * 128].rearrange("p (a b) -> p a b", a=kn)
                        dst_ap = XsT[b][:, k0:k0 + kn, sub * 128:(sub + 1) * 128]
                        if ev_alt[0]:
                            pb.op(act, [bk], [("XsT", b, sub, k0)], lambda e: e.activation(out=dst_ap, in_=src_ap, func=AF.Copy))
                        else:
                            pb.op(dve, [bk], [("XsT", b, sub, k0)], lambda e: e.tensor_copy(out=dst_ap, in_=src_ap))
            def p7_GU(j):
                b = j % 2
                xk = [("XsT", b, sub, k0) for sub in range(SUB) for k0 in range(0, KD, 8)]
                for fc in range(FC):
                    ba, bka = ps_next()
                    pb.mm(xk + [("W", b)], [bka],
                          [lambda e, kc=kc: e.matmul(psum[:, ba, 0:TS], Wg[b][:, kc, fc * 128:(fc + 1) * 128], XsT[b][:, kc, :],
                                                     start=(kc == 0), stop=(kc == KD - 1)) for kc in range(KD)])
                    si = sa_ring.next()
                    pb.op(act, [bka], [("sa", si)], lambda e: e.activation(out=sa[si][:], in_=psum[:, ba, 0:TS], func=AF.Silu))
                    bu, bku = ps_next()
                    pb.mm(xk + [("W", b)], [bku],
                          [lambda e, kc=kc: e.matmul(psum[:, bu, 0:TS], Wu[b][:, kc, fc * 128:(fc + 1) * 128], XsT[b][:, kc, :],
                                                     start=(kc == 0), stop=(kc == KD - 1)) for kc in range(KD)])
                    pb.op(dve, [bku, ("sa", si)], [("hT", b, fc)],
                          lambda e: e.tensor_tensor(out=hT[b][:, fc, :], in0=psum[:, bu, 0:TS], in1=sa[si][:], op=ALU.mult))
            def p7_D(j):
                b = j % 2
                hk = [("hT", b, fc) for fc in range(FC)]
                for sub in range(SUB):
                    yi = ys_ring.next()
                    for n in range(NQ):
                        bo, bko = ps_next()
                        pb.mm(hk + [("W", b)], [bko],
                              [lambda e, fc=fc: e.matmul(psum[:, bo, 0:qw], hT[b][:, fc, sub * 128:(sub + 1) * 128],
                                                         Wd[b][:, fc, n * qw:(n + 1) * qw], start=(fc == 0), stop=(fc == FC - 1))
                               for fc in range(FC)])
                        if n % 2 == 0:
                            pb.op(act, [bko], [("ysb", yi)],
                                  lambda e: e.activation(out=ysb[yi][:, n * qw:(n + 1) * qw], in_=psum[:, bo, 0:qw], func=AF.Copy))
                        else:
                            pb.op(dve, [bko], [("ysb", yi)],
                                  lambda e: e.tensor_copy(out=ysb[yi][:, n * qw:(n + 1) * qw], in_=psum[:, bo, 0:qw]))
                    r0 = j * TS + sub * 128
                    pb.dma(sp, ys_sc[yi], [("ysb", yi)], [], lambda e: e.dma_start(out=Ys[r0:r0 + 128, :], in_=ysb[yi][:]))

            p7_xload(0)
            p7_T(0)
            for j in range(NTILE):
                if 1 <= j and j + 1 < NTILE:
                    p7_W(j + 1)
                p7_GU(j)
                if j + 1 < NTILE:
                    p7_T(j + 1)
                p7_D(j)
            for e_ in pb.engs:
                e_.wait([(sc_, sc_.n) for sc_ in w_sc])
            pb.barrier()
        p67.close()

        with ExitStack() as p8:
            y1 = [sbuf(p8, "y1_%d" % i, [128, D], F32) for i in range(2)]
            y2 = [sbuf(p8, "y2_%d" % i, [128, D], F32) for i in range(2)]
            xf = [sbuf(p8, "xf%d" % i, [128, D], F32) for i in range(2)]
            ho = [sbuf(p8, "ho%d" % i, [128, D], F32) for i in range(2)]
            y1_sc = [pb.new_dma_sc("y1_%d" % i) for i in range(2)]
            y2_sc = [pb.new_dma_sc("y2_%d" % i) for i in range(2)]
            xf_sc = [pb.new_dma_sc("xf%d" % i) for i in range(2)]
            out_sc = [pb.new_dma_sc("out%d" % i) for i in range(2)]
            st6c = sbuf(p8, "st6c", [128, max(1, D // 512), 6], F32)
            mv4c = sbuf(p8, "mv4c", [128, 4], F32)
            c3_sc = pb.new_dma_sc("c3")
            pb.dma(sp, c3_sc, [], ["gln"], lambda e: e.dma_start(out=gln[:], in_=ln2_g))
            pb.dma(sp, c3_sc, [], ["bln"], lambda e: e.dma_start(out=bln[:], in_=ln2_b))
            c3_tok = (c3_sc, c3_sc.n)
            pb.deps["gln"].w = c3_tok
            pb.deps["bln"].w = c3_tok
            def p8_loads(tt):
                b = tt % 2
                pb.dma(pool, y1_sc[b], [], [("y1", b)],
                       lambda e: e.indirect_dma_start(out=y1[b][:], out_offset=None, in_=Ys,
                                                      in_offset=bass.IndirectOffsetOnAxis(ap=sli[:, tt, 0:1], axis=0)))
                pb.dma(pool, y2_sc[b], [], [("y2", b)],
                       lambda e: e.indirect_dma_start(out=y2[b][:], out_offset=None, in_=Ys,
                                                      in_offset=bass.IndirectOffsetOnAxis(ap=sli[:, tt, 1:2], axis=0)))
                pb.dma(sp, xf_sc[b], [], [("xf", b)], lambda e: e.dma_start(out=xf[b][:], in_=X1F[tt * 128:(tt + 1) * 128, :]))

            p8_loads(0)
            for tt in range(NT):
                b = tt % 2
                if tt + 1 < NT:
                    p8_loads(tt + 1)
                pb.op(act, [("xf", b)], [("xf", b)], lambda e: e.activation(out=xf[b][:], in_=xf[b][:], func=AF.Copy, scale=ALPHA))
                pb.op(dve, [("xf", b), ("y1", b)], [("xf", b)],
                      lambda e: e.scalar_tensor_tensor(out=xf[b][:], in0=y1[b][:], scalar=w12[:, tt, 0:1], in1=xf[b][:],
                                                       op0=ALU.mult, op1=ALU.add))
                pb.op(dve, [("xf", b), ("y2", b)], [("xf", b)],
                      lambda e: e.scalar_tensor_tensor(out=xf[b][:], in0=y2[b][:], scalar=w12[:, tt, 1:2], in1=xf[b][:],
                                                       op0=ALU.mult, op1=ALU.add))
                ln_rows(xf[b][:], ho[b][:], ("xf", b), ("ho", b), st6c, mv4c, gln[:], bln[:], ["gln", "bln"], mul_eng=dve)
                pb.dma(sp, out_sc[b], [("ho", b)], [], lambda e: e.dma_start(out=out[tt * 128:(tt + 1) * 128, :], in_=ho[b][:]))
            sp.wait([(sc_, sc_.n) for sc_ in out_sc])
            pb.barrier()
    return nc, c


def const_tables(c):
    NG, HPG = c["NG"], c["HPG"]
    n = NG * HPG
    slopes = (2.0 ** (-ALIBI_MAX * np.arange(1, n + 1, dtype=np.float32) / n)).reshape(NG, HPG)
    k = np.arange(128)[:, None].astype(np.float64)
    q = np.arange(128)[None, :].astype(np.float64)
    ebt = np.zeros((n, 128, 2, 128), np.float32)
    for g, (window, dil) in enumerate(c["PATTERNS"]):
        assert window // dil == 128
        for hh in range(HPG):
            s = float(slopes[g, hh]) * dil
            cur = np.where(k <= q, np.exp(-s * (q - k)), 0.0)
            prev = np.where(k >= q, np.exp(-s * (q + 128 - k)), 0.0)
            ebt[g * HPG + hh, :, 0, :] = cur
            ebt[g * HPG + hh, :, 1, :] = prev
    tri = (np.arange(128)[:, None] < np.arange(128)[None, :]).astype(np.float32)
    return dict(
        ident_f=np.eye(128, dtype=np.float32),
        ident_b=np.eye(128, dtype=np.float32).astype(ml_dtypes.bfloat16),
        ebt=ebt, tri=tri,
        iota_e=np.broadcast_to(np.arange(c["NE"], dtype=np.float32), (128, c["NE"])).copy(),
        iota_t=np.broadcast_to(np.arange(c["NTILE"], dtype=np.float32), (128, c["NTILE"])).copy(),
        iota_p=np.arange(128, dtype=np.float32).reshape(128, 1).copy(),
    )


def ex_layout(w):
    ne, k, n = w.shape
    return np.ascontiguousarray(w.reshape(ne, k // 128, 128, n).transpose(0, 2, 1, 3)).reshape(ne * 128, (k // 128) * n)


def prep_shared(c, inp):
    f = lambda a: np.ascontiguousarray(np.asarray(a, dtype=np.float32))
    rep = lambda v: np.ascontiguousarray(np.broadcast_to(f(v).reshape(1, -1), (128, f(v).size)))
    L = 0
    G, E, D = c["G"], c["E"], c["D"]
    w_r = np.concatenate([f(inp["w_group"])[L], f(inp["w_router"])[L].transpose(1, 0, 2).reshape(D, G * E)], axis=1)
    b_r = np.concatenate([f(inp["b_group"])[L].reshape(-1), f(inp["b_router"])[L].reshape(-1)])
    sh = dict(
        lnm_g=rep(inp["ln_mem_g"]), lnm_b=rep(inp["ln_mem_b"]),
        w_in=f(inp["w_in"])[L],
        b_col=np.ascontiguousarray(f(inp["b_in"])[L].reshape(-1, 128).T),
        wconv_col=np.ascontiguousarray(f(inp["w_conv"])[L].reshape(3, -1, 128).transpose(2, 0, 1)),
        w_conv_out=f(inp["w_conv_out"])[L], w_dil_out=f(inp["w_dil_out"])[L], w_mem_kv=f(inp["w_mem_kv"])[L],
        w_mem_out=f(inp["w_mem_out"])[L], w_o=f(inp["w_o"])[L],
        ln1_g=rep(f(inp["ln1_g"])[L]), ln1_b=rep(f(inp["ln1_b"])[L]),
        w_r=np.ascontiguousarray(w_r), b_r=rep(b_r),
        w_gate=ex_layout(f(inp["w_gate"])[L].reshape(G * E, D, c["F"])),
        w_up=ex_layout(f(inp["w_up"])[L].reshape(G * E, D, c["F"])),
        w_down=ex_layout(f(inp["w_down"])[L].reshape(G * E, c["F"], D)),
        ln2_g=rep(f(inp["ln2_g"])[L]), ln2_b=rep(f(inp["ln2_b"])[L]),
    )
    sh.update(const_tables(c))
    return sh


def make_in_maps(c, inp):
    sh = prep_shared(c, inp)
    xs = np.asarray(inp["x"], dtype=np.float32)
    ms = np.asarray(inp["mem"], dtype=np.float32)
    maps = []
    for b in range(xs.shape[0]):
        m = dict(sh)
        m["x"] = np.ascontiguousarray(xs[b])
        m["mem"] = np.ascontiguousarray(ms[b])
        maps.append(m)
    return maps


def kernel(**inputs):
    c = derive(FULL)
    nc, _ = build_program(FULL)
    maps = make_in_maps(c, inputs)
    res = run_bass_kernel_spmd(nc, maps, core_ids=list(range(len(maps))))
    return np.stack([np.asarray(r["out"], dtype=np.float32) for r in res.results], axis=0)
```
